# Optimizing a Trainium2 kernel written in Bass

```python
import math
import jax, jax.numpy as jnp
from jax import lax
import numpy as np

D_MODEL = 2048
BATCH = 16
SEQ = 2048
DEPTH = 2

D_MIX = D_MODEL
DA_WIDTH = D_MIX // 2
DA_HEADS = 8
DA_HEAD_V = DA_WIDTH // DA_HEADS
DA_HEAD_QK = DA_HEAD_V // 2
GLA_WIDTH = D_MIX - DA_WIDTH
GLA_HEADS = 4
GLA_KEY_DIM = GLA_WIDTH // 2
GLA_HEAD_K = GLA_KEY_DIM // GLA_HEADS
GLA_HEAD_V = GLA_WIDTH // GLA_HEADS
GLA_GATE_RANK = 16
GLA_GATE_NORMALIZER = 16.0
GLA_CHUNK = 64
Q_BLOCK = 128
D_FF = ((8 * D_MODEL // 3 + 255) // 256) * 256
CONV_WIDTH = 3
NORM_EPS = 1e-6
N_MOD = 6
PROJ_SIZES = (DA_WIDTH, DA_WIDTH, DA_WIDTH,
              GLA_KEY_DIM, GLA_KEY_DIM, GLA_WIDTH,
              GLA_WIDTH, GLA_GATE_RANK)
N_PROJ = sum(PROJ_SIZES)
PROJ_SPLITS = tuple(int(s) for s in np.cumsum(PROJ_SIZES)[:-1])

kernel_name = "hybrid_diffattn_gla_convffn_adaln"


def _alibi_slopes(n):
    start = 2.0 ** (-8.0 / n)
    return np.array([start ** (i + 1) for i in range(n)], dtype=np.float32)


def _lambda_init(layer_idx):
    return 0.8 - 0.6 * math.exp(-0.3 * layer_idx)


def rms_norm(x, g):
    xf = x.astype(jnp.float32)
    y = xf * lax.rsqrt(jnp.mean(xf * xf, axis=-1, keepdims=True) + NORM_EPS)
    return (y * g.astype(jnp.float32)).astype(x.dtype)


def diff_attention(q, k, v, lam, slopes):
    T = q.shape[3]
    scale = DA_HEAD_QK ** -0.5
    outs = []
    for i in range(T // Q_BLOCK):
        q0, q1 = i * Q_BLOCK, (i + 1) * Q_BLOCK
        qb = q[:, :, :, q0:q1]
        kb = k[:, :, :, :q1]
        vb = v[:, :, :q1]
        s = jnp.einsum('bhmqd,bhmkd->bhmqk', qb, kb).astype(jnp.float32) * scale
        dist = (jnp.arange(q0, q1)[:, None] - jnp.arange(q1)[None, :]).astype(jnp.float32)
        bias = jnp.where(dist[None] >= 0, -slopes[:, None, None] * dist[None], -jnp.inf)
        p = jax.nn.softmax(s + bias[None, :, None], axis=-1)
        pd = p[:, :, 0] - lam * p[:, :, 1]
        outs.append(jnp.einsum('bhqk,bhkd->bhqd', pd.astype(vb.dtype), vb))
    return jnp.concatenate(outs, axis=2)


def gla_chunked(q, k, v, log_a):
    B, H, T, dk = q.shape
    dv = v.shape[-1]
    nc = T // GLA_CHUNK

    def to_chunks(a):
        return jnp.moveaxis(a.reshape(B, H, nc, GLA_CHUNK, a.shape[-1]), 2, 0)

    qc, kc, vc = (to_chunks(a.astype(jnp.float32)) for a in (q, k, v))
    bc = jnp.cumsum(to_chunks(log_a.astype(jnp.float32)), axis=-2)
    causal = jnp.tril(jnp.ones((GLA_CHUNK, GLA_CHUNK), dtype=bool))

    def step(S, inp):
        qi, ki, vi, bi = inp
        diff = bi[:, :, :, None, :] - bi[:, :, None, :, :]
        decay = jnp.exp(jnp.where(causal[None, None, :, :, None], diff, -jnp.inf))
        A = jnp.einsum('bhtsd,bhsd->bhts', qi[:, :, :, None, :] * decay, ki)
        o = jnp.einsum('bhts,bhsv->bhtv', A, vi) \
            + jnp.einsum('bhtd,bhdv->bhtv', qi * jnp.exp(bi), S)
        b_last = bi[:, :, -1:, :]
        S_new = jnp.exp(b_last[:, :, 0, :])[..., None] * S \
            + jnp.einsum('bhsd,bhsv->bhdv', ki * jnp.exp(b_last - bi), vi)
        return S_new, o

    S0 = jnp.zeros((B, H, dk, dv), jnp.float32)
    _, oc = lax.scan(step, S0, (qc, kc, vc, bc))
    return jnp.moveaxis(oc, 0, 2).reshape(B, H, T, dv)


def causal_dwconv(x, w, b):
    T = x.shape[1]
    xp = jnp.pad(x, ((0, 0), (CONV_WIDTH - 1, 0), (0, 0)))
    y = b.astype(x.dtype)
    for j in range(CONV_WIDTH):
        y = y + xp[:, j:j + T] * w[j].astype(x.dtype)
    return y


def setup_inputs(seed: int = 0) -> dict:
    key = jax.random.key(seed)
    ks = jax.random.split(key, 20)
    n = jax.random.normal
    f = jnp.float32
    return {
        "x": n(ks[0], (BATCH, SEQ, D_MODEL), f),
        "c": n(ks[1], (BATCH, D_MODEL), f),
        "w_ada": n(ks[2], (DEPTH, D_MODEL, N_MOD * D_MODEL), f) * D_MODEL ** -0.5,
        "b_ada": n(ks[3], (DEPTH, N_MOD * D_MODEL), f) * 0.02,
        "g_mix_norm": 1.0 + 0.02 * n(ks[4], (DEPTH, D_MODEL), f),
        "w_in": n(ks[5], (DEPTH, D_MODEL, N_PROJ), f) * D_MODEL ** -0.5,
        "diff_lambda": 0.1 * n(ks[6], (DEPTH, 4, DA_HEAD_QK), f),
        "g_diff_subln": 1.0 + 0.02 * n(ks[7], (DEPTH, DA_HEAD_V), f),
        "w_gk_up": n(ks[8], (DEPTH, GLA_GATE_RANK, GLA_KEY_DIM), f) * GLA_GATE_RANK ** -0.5,
        "b_gk": 0.02 * n(ks[9], (DEPTH, GLA_KEY_DIM), f),
        "g_gla_norm": 1.0 + 0.02 * n(ks[10], (DEPTH, GLA_HEAD_V), f),
        "w_o": n(ks[11], (DEPTH, D_MIX, D_MODEL), f) * D_MIX ** -0.5,
        "g_ffn_norm": 1.0 + 0.02 * n(ks[12], (DEPTH, D_MODEL), f),
        "w_up": n(ks[13], (DEPTH, D_MODEL, 2 * D_FF), f) * D_MODEL ** -0.5,
        "w_conv": n(ks[14], (DEPTH, CONV_WIDTH, D_FF), f) * CONV_WIDTH ** -0.5,
        "b_conv": 0.02 * n(ks[15], (DEPTH, D_FF), f),
        "w_down": n(ks[16], (DEPTH, D_FF, D_MODEL), f) * D_FF ** -0.5,
        "g_final": 1.0 + 0.02 * n(ks[17], (D_MODEL,), f),
    }


def reference(x, c, w_ada, b_ada, g_mix_norm, w_in, diff_lambda, g_diff_subln, w_gk_up, b_gk,
              g_gla_norm, w_o, g_ffn_norm, w_up, w_conv, b_conv, w_down, g_final):
    B, T, _ = x.shape
    slopes = jnp.asarray(_alibi_slopes(DA_HEADS))
    c_act = jax.nn.silu(c)
    for l in range(DEPTH):
        mod = (c_act @ w_ada[l] + b_ada[l])[:, None, :]
        sh1, sc1, gt1, sh2, sc2, gt2 = jnp.split(mod, N_MOD, axis=-1)

        h = rms_norm(x, g_mix_norm[l]) * (1.0 + sc1) + sh1
        proj = h @ w_in[l]
        dq, dk, dv, gq, gk, gv, gg, g_low = jnp.split(proj, PROJ_SPLITS, axis=-1)

        lam_init = _lambda_init(l)
        lq1, lk1, lq2, lk2 = (diff_lambda[l, i].astype(jnp.float32) for i in range(4))
        lam = jnp.exp(jnp.sum(lq1 * lk1)) - jnp.exp(jnp.sum(lq2 * lk2)) + lam_init
        qd = dq.reshape(B, T, DA_HEADS, 2, DA_HEAD_QK).transpose(0, 2, 3, 1, 4)
        kd = dk.reshape(B, T, DA_HEADS, 2, DA_HEAD_QK).transpose(0, 2, 3, 1, 4)
        vd = dv.reshape(B, T, DA_HEADS, DA_HEAD_V).transpose(0, 2, 1, 3)
        od = diff_attention(qd, kd, vd, lam, slopes)
        od = rms_norm(od, g_diff_subln[l]) * (1.0 - lam_init)
        od = od.transpose(0, 2, 1, 3).reshape(B, T, DA_WIDTH)

        log_a = jax.nn.log_sigmoid((g_low @ w_gk_up[l] + b_gk[l]).astype(jnp.float32)) / GLA_GATE_NORMALIZER
        qg = (gq * GLA_HEAD_K ** -0.5).reshape(B, T, GLA_HEADS, GLA_HEAD_K).transpose(0, 2, 1, 3)
        kg = gk.reshape(B, T, GLA_HEADS, GLA_HEAD_K).transpose(0, 2, 1, 3)
        vg = gv.reshape(B, T, GLA_HEADS, GLA_HEAD_V).transpose(0, 2, 1, 3)
        ag = log_a.reshape(B, T, GLA_HEADS, GLA_HEAD_K).transpose(0, 2, 1, 3)
        og = gla_chunked(qg, kg, vg, ag).astype(x.dtype).transpose(0, 2, 1, 3)
        og = rms_norm(og, g_gla_norm[l]) * jax.nn.silu(gg.reshape(B, T, GLA_HEADS, GLA_HEAD_V))
        og = og.reshape(B, T, GLA_WIDTH)

        mix = jnp.concatenate([od, og], axis=-1) @ w_o[l]
        x = x + gt1 * mix

        h2 = rms_norm(x, g_ffn_norm[l]) * (1.0 + sc2) + sh2
        u, g = jnp.split(h2 @ w_up[l], 2, axis=-1)
        act = jax.nn.gelu(causal_dwconv(g, w_conv[l], b_conv[l]), approximate=False) * u
        x = x + gt2 * (act @ w_down[l])
    return rms_norm(x, g_final)
```

```python
import numpy as np
import concourse.bass as bass
import concourse.mybir as mybir

F32 = mybir.dt.float32
BF16 = mybir.dt.bfloat16
AF = mybir.ActivationFunctionType
ALU = mybir.AluOpType
AX = mybir.AxisListType

CELL = 64
_DS = {F32: 4, BF16: 2}


def _dsize(dt):
    return _DS.get(dt, 4)


class Prog:
    ENG = ("pe", "act", "dve", "pool", "sp")

    def __init__(self, nc, sb_bytes, same_engine_sync=True):
        self.nc = nc
        self.ops = {e: [] for e in self.ENG}
        self.trk = {}
        self.cnt = []
        self.sig = []
        self.is_chan = []
        for e in ("pe", "act", "dve", "pool"):
            self._tracker(e, False)
        self.ncell = {"sb": sb_bytes // CELL, "ps": 16384 // CELL}
        self.MAXT = 120
        self.lw = {k: np.zeros((self.MAXT, n), np.int32) for k, n in self.ncell.items()}
        self.lr = {k: np.zeros((self.MAXT, n), np.int32) for k, n in self.ncell.items()}
        self.dram = {}
        self.bank_last = np.zeros((self.MAXT, 8), np.int32)
        self.same = same_engine_sync
        self.waited = {e: {} for e in self.ENG}

    def _tracker(self, name, is_chan):
        if name not in self.trk:
            self.trk[name] = len(self.cnt)
            self.cnt.append(0)
            self.sig.append(set())
            self.is_chan.append(is_chan)
        return self.trk[name]

    @staticmethod
    def cells(ap):
        ds = _dsize(ap.dtype)
        dims = ap.ap
        pstep = dims[0][0]
        off = (int(ap.offset) % pstep) * ds if pstep > 0 else int(ap.offset) * ds
        space = "ps" if "PSum" in type(ap.tensor).__name__ else "sb"
        starts = np.array([off], np.int64)
        inner = ds
        fd = [(s, c) for (s, c) in dims[1:] if c > 1 and s != 0]
        fd.sort(key=lambda x: x[0])
        rest = []
        for s, c in fd:
            if s * ds == inner:
                inner *= c
            else:
                rest.append((s, c))
        for s, c in rest:
            starts = (starts[:, None] + (np.arange(c, dtype=np.int64) * s * ds)[None, :]).ravel()
        lo = starts // CELL
        hi = (starts + inner - 1) // CELL
        n = int((hi - lo).max()) + 1
        cs = (lo[:, None] + np.arange(n)[None, :])
        cs = np.minimum(cs, hi[:, None]).ravel()
        return space, np.unique(cs)

    def _deps(self, reads, writes, dreads, dwrites, me):
        deps = np.zeros(self.MAXT, np.int32)
        acc = []
        bdeps = np.zeros(self.MAXT, np.int32)
        for ap in reads:
            sp, cs = self.cells(ap)
            acc.append((sp, cs, False))
            np.maximum(deps, self.lw[sp][:, cs].max(axis=1), out=deps)
            if sp == "ps":
                np.maximum(bdeps, self.bank_last[:, np.unique(cs // (2048 // CELL))].max(axis=1), out=bdeps)
        for ap in writes:
            sp, cs = self.cells(ap)
            acc.append((sp, cs, True))
            np.maximum(deps, self.lw[sp][:, cs].max(axis=1), out=deps)
            np.maximum(deps, self.lr[sp][:, cs].max(axis=1), out=deps)
            if sp == "ps":
                np.maximum(bdeps, self.bank_last[:, np.unique(cs // (2048 // CELL))].max(axis=1), out=bdeps)
        bdeps[me] = 0
        np.maximum(deps, bdeps, out=deps)
        d = {int(t): int(deps[t]) for t in np.nonzero(deps)[0]}
        for k in dreads:
            rec = self.dram.setdefault(k, ({}, {}))
            for t, i in rec[0].items():
                d[t] = max(d.get(t, 0), i)
        for k in dwrites:
            rec = self.dram.setdefault(k, ({}, {}))
            for t, i in list(rec[0].items()) + list(rec[1].items()):
                d[t] = max(d.get(t, 0), i)
        return d, acc

    def _commit(self, t, idx, acc, dreads, dwrites):
        for sp, cs, w in acc:
            (self.lw if w else self.lr)[sp][t, cs] = idx
            if sp == "ps":
                self.bank_last[t, np.unique(cs // (2048 // CELL))] = idx
        for k in dreads:
            self.dram[k][1][t] = idx
        for k in dwrites:
            self.dram[k][0][t] = idx
            self.dram[k][1].clear()

    def op(self, eng, fn, reads=(), writes=(), dreads=(), dwrites=()):
        t = self.trk[eng]
        d, acc = self._deps(reads, writes, dreads, dwrites, t)
        if eng == "pe" or not self.same:
            d.pop(t, None)
        self.cnt[t] += 1
        idx = self.cnt[t]
        waits = self._waits(eng, d)
        self.ops[eng].append((fn, waits, t, idx))
        self._commit(t, idx, acc, dreads, dwrites)

    def dma(self, queue, chan, fn, reads=(), writes=(), dreads=(), dwrites=()):
        t = self._tracker("ch:" + chan, True)
        d, acc = self._deps(reads, writes, dreads, dwrites, t)
        d.pop(t, None)
        self.cnt[t] += 1
        idx = self.cnt[t]
        waits = self._waits(queue, d)
        self.ops[queue].append((fn, waits, t, idx))
        self._commit(t, idx, acc, dreads, dwrites)

    def _waits(self, eng, d):
        w = []
        wd = self.waited[eng]
        for t, i in d.items():
            if wd.get(t, 0) >= i:
                continue
            wd[t] = i
            self.sig[t].add(i)
            w.append((t, i))
        return w

    def wait_all(self, eng):
        d = {t: c for t, c in enumerate(self.cnt) if c > 0}
        d.pop(self.trk.get(eng, -1), None)
        waits = self._waits(eng, d)
        self.ops[eng].append((None, waits, None, None))

    def emit(self):
        nc = self.nc
        nt = len(self.cnt)
        rank = []
        for t in range(nt):
            s = sorted(self.sig[t])
            rank.append({i: k + 1 for k, i in enumerate(s)})
        import contextlib
        with contextlib.ExitStack() as st:
            sems = [st.enter_context(nc.semaphore(f"s{t}")) for t in range(nt)]
            block = st.enter_context(nc.Block())

            def run(eng_name, e):
                for fn, waits, t, idx in self.ops[eng_name]:
                    for (wt, wi) in waits:
                        v = 16 * wi if self.is_chan[wt] else rank[wt][wi]
                        e.wait_ge(sems[wt], v)
                    if fn is None:
                        continue
                    ins = fn(e)
                    if self.is_chan[t]:
                        ins.then_inc(sems[t], 16)
                    elif idx in rank[t]:
                        ins.then_inc(sems[t], 1)

            @block.tensor
            def _(e):
                run("pe", e)

            @block.scalar
            def _(e):
                run("act", e)

            @block.vector
            def _(e):
                run("dve", e)

            @block.gpsimd
            def _(e):
                run("pool", e)

            @block.sync
            def _(e):
                run("sp", e)
        return {e: len(v) for e, v in self.ops.items()}


import math, os
from concourse.bass_utils import run_bass_kernel_spmd

D = 2048; T = 2048; NSQ = 2; NTOK = NSQ * T; KC = 16; DFF = 5632; JC = 44
NPROJ = 6160; EPS = 1e-6
KB_ = 1024


def _lam_init(l):
    return 0.8 - 0.6 * math.exp(-0.3 * l)


def build_program(stop_after=None):
    nc = bass.Bass("TRN2", target_bir_lowering=False)
    dt_in = lambda n, s: nc.dram_tensor(n, s, F32, kind="ExternalInput").ap()
    x_d = dt_in("x", [NTOK, D]); c_d = dt_in("c", [NSQ, D])
    w_ada = dt_in("w_ada", [2, D, 6 * D]); b_ada = dt_in("b_ada", [2, 6 * D])
    g_mix = dt_in("g_mix_norm", [2, D]); w_in = dt_in("w_in", [2, D, NPROJ])
    dlam = dt_in("diff_lambda", [2, 256]); g_sub = dt_in("g_diff_subln", [2, 128])
    w_gk = dt_in("w_gk_up", [2, 16, 512]); b_gk = dt_in("b_gk", [2, 512])
    g_gla = dt_in("g_gla_norm", [2, 256]); w_o = dt_in("w_o", [2, D, D])
    g_ffn = dt_in("g_ffn_norm", [2, D]); w_up = dt_in("w_up", [2, D, 2 * DFF])
    w_cv = dt_in("w_conv", [2, 3, DFF]); b_cv = dt_in("b_conv", [2, DFF])
    w_dn = dt_in("w_down", [2, DFF, D]); g_fin = dt_in("g_final", [D])
    cst_d = dt_in("consts", [128, 512])
    y_d = nc.dram_tensor("y", [NTOK, D], F32, kind="ExternalOutput").ap()
    xT_d = nc.dram_tensor("xT_s", [D, NTOK], F32, kind="Internal").ap()
    mix_d = nc.dram_tensor("mix_s", [D, NTOK], BF16, kind="Internal").ap()
    act_d = nc.dram_tensor("act_s", [DFF, NTOK], BF16, kind="Internal").ap()
    dbg_d = None
    if stop_after is not None:
        dbg_d = nc.dram_tensor("dbg", [D, NTOK], F32, kind="ExternalOutput").ap()
    xT_v = xT_d.rearrange("(k p) t -> p k t", p=128)
    mix_v = mix_d.rearrange("(k p) t -> p k t", p=128)
    act_v = act_d.rearrange("(j p) t -> p j t", p=128)

    SB = 204 * KB_
    import contextlib
    with contextlib.ExitStack() as st:
        arena = st.enter_context(nc.sbuf_tensor("arena", [128, SB // 4], F32))
        psum = st.enter_context(nc.psum_tensor("psum", [128, 4096], F32))
        P = Prog(nc, SB)

        class Region:
            def __init__(s, lo, hi):
                s.lo, s.hi, s.o = lo, hi, lo

            def reset(s):
                s.o = s.lo

            def a(s, nbytes, dt=F32, pat=None, **kw):
                n = (nbytes + 63) // 64 * 64
                assert s.o + n <= s.hi, ("region overflow", s.lo, s.hi, s.o, n)
                v = arena[:, s.o // 4:(s.o + nbytes + 3) // 4]
                s.o += n
                if dt == BF16:
                    v = v.bitcast(BF16)[:, 0:nbytes // 2]
                else:
                    v = v[:, 0:nbytes // 4]
                if pat:
                    v = v.rearrange(pat, **kw)
                return v

        RC = Region(0, 12 * KB_); RH = Region(12 * KB_, 76 * KB_); RW = Region(76 * KB_, 100 * KB_)
        RHW = Region(12 * KB_, 100 * KB_)
        RD = Region(100 * KB_, 144 * KB_); RG = Region(144 * KB_, 196 * KB_); RM = Region(196 * KB_, 204 * KB_)

        _rr = {"big": 0, "small": 0, "mid": 0}

        def big():
            i = _rr["big"]; _rr["big"] = (i + 1) % 4
            return psum[:, i * 512:(i + 1) * 512]

        def small():
            i = _rr["big"]; _rr["big"] = (i + 1) % 4
            return psum[:, i * 512:i * 512 + 128]

        def mid():
            i = _rr["mid"]; _rr["mid"] = (i + 1) % 4
            return psum[:, 2048 + i * 512:2048 + i * 512 + 256]

        def bf(ps):
            return ps.bitcast(BF16)

        def mm(out, lhsT, rhs, start, stop):
            P.op("pe", lambda e: e.matmul(out, lhsT=lhsT, rhs=rhs, start=start, stop=stop),
                 reads=[lhsT, rhs], writes=[out])

        def tp(out, in_, ident):
            P.op("pe", lambda e: e.transpose(out=out, in_=in_, identity=ident), reads=[in_, ident], writes=[out])

        def act(out, in_, func, bias=None, scale=None, accum=None):
            rd = [in_] + [a for a in (bias, scale) if a is not None and not isinstance(a, float)]
            wr = [out] + ([accum] if accum is not None else [])
            kw = {}
            if bias is not None: kw["bias"] = bias
            if scale is not None: kw["scale"] = scale
            if accum is not None: kw["accum_out"] = accum
            P.op("act", lambda e: e.activation(out=out, in_=in_, func=func, **kw), reads=rd, writes=wr)

        def vcopy(out, in_, eng="dve"):
            P.op(eng, lambda e: e.tensor_copy(out=out, in_=in_), reads=[in_], writes=[out])

        def vtt(out, a, b, op, eng="dve"):
            P.op(eng, lambda e: e.tensor_tensor(out=out, in0=a, in1=b, op=op), reads=[a, b], writes=[out])

        def vts(out, a, s1, s2, op0, op1, eng="dve"):
            rd = [a] + [s for s in (s1, s2) if s is not None and not isinstance(s, float)]
            P.op(eng, lambda e: e.tensor_scalar(out=out, in0=a, scalar1=s1, scalar2=s2, op0=op0, op1=op1),
                 reads=rd, writes=[out])

        def vstt(out, a, s, b, op0, op1, eng="dve"):
            rd = [a, b] + ([s] if not isinstance(s, float) else [])
            P.op(eng, lambda e: e.scalar_tensor_tensor(out=out, in0=a, scalar=s, in1=b, op0=op0, op1=op1),
                 reads=rd, writes=[out])

        def vrecip(out, in_):
            P.op("dve", lambda e: e.reciprocal(out=out, in_=in_), reads=[in_], writes=[out])

        def vmemset(ap, val, eng="dve"):
            P.op(eng, lambda e: e.memset(ap, val), writes=[ap])

        def ld(chan, out, in_, dreads=(), slow=False):
            if slow:
                P.dma("sp", chan, lambda e: e.dma_start(out=out, in_=in_, allow_slow_non_contiguous=True),
                      writes=[out], dreads=dreads)
            else:
                P.dma("sp", chan, lambda e: e.dma_start(out=out, in_=in_), writes=[out], dreads=dreads)

        def ldc(chan, out, in_):
            P.dma("pool", chan, lambda e: e.dma_start(out=out, in_=in_), writes=[out])

        def stt_(chan, out, in_, dwrites=()):
            P.dma("sp", chan, lambda e: e.dma_start(out=out, in_=in_), reads=[in_], dwrites=dwrites)

        evac_rr = [0]

        def evac(out, in_, scale=None):
            evac_rr[0] ^= 1
            if scale is not None:
                act(out, in_, AF.Copy, scale=scale)
            elif evac_rr[0]:
                act(out, in_, AF.Copy)
            else:
                vcopy(out, in_)

        def bc_mid(ap, n):
            return bass.AP(ap.tensor, ap.offset, [list(ap.ap[0]), [0, n], list(ap.ap[1])])

        cst = RC.a(2048, F32)
        ident_f = cst[:, 0:128]; tri_f = cst[:, 128:256]; U_f = cst[:, 256:384]; btab = cst[:, 384:512]
        ident_b = RC.a(256, BF16); tri_b = RC.a(256, BF16); ones_b = RC.a(256, BF16)
        modT = RC.a(2 * 96 * 2 * 4, F32, "p (l j b) -> p l j b", l=2, j=96)
        gmix = RC.a(64); gffn = RC.a(64); gfin = RC.a(64)
        gsc = RC.a(2 * 16 * 2 * 4, F32, "p (w k b) -> p w k b", w=2, k=16)
        wcv = RC.a(3 * JC * 4, F32, "p (j k) -> p j k", j=3); bcv = RC.a(JC * 4)
        gsub_bc = RC.a(512); ggla_bc = RC.a(1024); dl = RC.a(1024)
        lamt = RC.a(64)
        wgk = RC.a(1024, BF16); bgk = RC.a(1024, BF16)
        cf = RC.a(128, F32, "p (k b) -> p k b", k=16); cb = RC.a(64, BF16, "p (k b) -> p k b", k=16)
        stat = RC.a(512)

        ld("cst", cst, cst_d)
        vcopy(ident_b, ident_f); vcopy(tri_b, tri_f); vmemset(ones_b, 1.0)
        ld("gfin", gfin, g_fin.rearrange("(k p) -> p k", p=128), slow=True)

        RG.reset()
        xtok = [RG.a(8192) for _ in range(2)]
        cstg = [RG.a(8192, F32, "p (k t) -> p k t", k=16) for _ in range(2)]

        def conv_block(blk):
            s, t5 = blk // 16, (blk % 16) // 4
            xt = xtok[blk % 2]; cs = cstg[blk % 2]
            ld(f"xtok{blk % 2}", xt, x_d[blk * 128:(blk + 1) * 128, :])
            for g in range(4):
                pb = big()
                for q in range(4):
                    k = g * 4 + q
                    tp(pb[:, q * 128:(q + 1) * 128], xt[:, k * 128:(k + 1) * 128], ident_f)
                evac(cs[:, g * 4:(g + 1) * 4, :], pb.rearrange("p (k t) -> p k t", k=4))
            stt_(f"cstg{blk % 2}", xT_v[:, :, blk * 128:(blk + 1) * 128], cs,
                 dwrites=[("xT", s, t5, f) for f in range(16)])

        class _Stop(Exception):
            pass

        def dump_and_finish():
            P.wait_all("sp")
            for r in range(16):
                P.dma("sp", f"dbg{r % 4}", lambda e, r=r: e.dma_start(out=dbg_d[r * 128:(r + 1) * 128, :], in_=xT_d[r * 128:(r + 1) * 128, :]))

        MIXSTOP = os.environ.get("MIXSTOP", "")

        def dbg_sb(src, row0, ncol):
            RM.reset()
            stg_ = RM.a(8192)
            for c0 in range(0, ncol, 2048):
                n_ = min(2048, ncol - c0)
                vcopy(stg_[:, 0:n_], src[:, c0:c0 + n_])
                stt_("dbgsb", dbg_d[row0:row0 + 128, c0:c0 + n_], stg_[:, 0:n_])

        def dbg_mix(row0, nrows):
            RW.reset()
            b16 = RW.a(4096, BF16)
            for r in range(row0, row0 + nrows, 128):
                P.wait_all("sp")
                ld("dbgm", b16, mix_d[r:r + 128, 0:2048])
                dbg_sb(b16, r, 2048)

        cb_ready = [False]

        def mod_group(l, g, wa, wchan, brow, bchan, mrow):
            ldc(wchan, wa, w_ada[l][:, g * 512:(g + 1) * 512].rearrange("(k p) n -> p k n", p=128))
            ld(bchan, brow[0:2, :], b_ada[l, g * 512:(g + 1) * 512].partition_broadcast(2))
            pb = big()
            for kc in range(KC):
                mm(pb[0:2, :], cb[:, kc, :], wa[:, kc, :], kc == 0, kc == KC - 1)
            vtt(mrow[0:2, :], pb[0:2, :], brow[0:2, :], ALU.add)
            pss = small()
            for q in range(4):
                mm(pss[:, q * 2:(q + 1) * 2], mrow[0:2, q * 128:(q + 1) * 128], ident_f[0:2, 0:2], True, True)
            vcopy(modT[:, l, g * 4:(g + 1) * 4, :], pss[:, 0:8].rearrange("p (q b) -> p q b", q=4))

        def mod_phase(with_conv, layers=(0,)):
            for b in range(NSQ):
                ld(f"cf{b}", cf[:, :, b], c_d[b].rearrange("(k p) -> p k", p=128), slow=True)
            act(cb, cf, AF.Silu)
            RH.reset(); RM.reset()
            was = [RH.a(16384, BF16, "p (k n) -> p k n", k=16) for _ in range(3)]
            mrow = RM.a(2048)
            brows = [RM.a(2048) for _ in range(2)]
            blk = 0
            n = 0
            ntot = 24 * len(layers)
            for l in layers:
                for g in range(24):
                    if with_conv:
                        while blk < NTOK // 128 and blk * ntot <= n * 32:
                            conv_block(blk); blk += 1
                    mod_group(l, g, was[n % 3], f"wa{n % 3}", brows[n % 2], f"brow{n % 2}", mrow)
                    n += 1
            if with_conv:
                while blk < NTOK // 128:
                    conv_block(blk); blk += 1

        def layer_setup(l):
            ld("gmix", gmix, g_mix[l].rearrange("(k p) -> p k", p=128), slow=True)
            ld("gffn", gffn, g_ffn[l].rearrange("(k p) -> p k", p=128), slow=True)
            for w, (gv_, j0) in enumerate(((gmix, 16), (gffn, 64))):
                for b in range(NSQ):
                    vts(gsc[:, w, :, b], modT[:, l, j0:j0 + 16, b], 1.0, None, ALU.add, ALU.bypass)
                    vtt(gsc[:, w, :, b], gsc[:, w, :, b], gv_, ALU.mult)
            for j in range(3):
                ld(f"wcv{j}", wcv[:, j, :], w_cv[l, j].rearrange("(k p) -> p k", p=128), slow=True)
            ld("bcv", bcv, b_cv[l].rearrange("(k p) -> p k", p=128), slow=True)
            ld("gsub", gsub_bc, g_sub[l].partition_broadcast(128))
            vts(gsub_bc, gsub_bc, float(1.0 - _lam_init(l)), None, ALU.mult, ALU.bypass)
            ld("ggla", ggla_bc, g_gla[l].partition_broadcast(128))
            ld("dl", dl, dlam[l].partition_broadcast(128))
            dl4 = dl.rearrange("p (a w d) -> p a w d", a=2, w=2)
            prod = stat[:, 0:128].rearrange("p (a d) -> p a d", a=2)
            vtt(prod, dl4[:, :, 0, :], dl4[:, :, 1, :], ALU.mult)
            P.op("dve", lambda e: e.reduce_sum(out=lamt[:, 0:2], in_=prod, axis=AX.X), reads=[prod], writes=[lamt[:, 0:2]])
            act(lamt[:, 2:4], lamt[:, 0:2], AF.Exp)
            vtt(lamt[:, 4:5], lamt[:, 3:4], lamt[:, 2:3], ALU.subtract)
            vts(lamt[:, 4:5], lamt[:, 4:5], float(-_lam_init(l)), None, ALU.add, ALU.bypass)
            ldc("wgk", wgk[0:16, :], w_gk[l])
            ldc("bgk", bgk[0:1, :], b_gk[l:l + 1, :])

        def rms_stats(xin, sq, rs, ntok, eps_scale):
            act(sq.rearrange("p k t -> p (k t)"), xin.rearrange("p k t -> p (k t)"), AF.Square)
            pm = mid()[:, 0:ntok]
            for k in range(KC):
                mm(pm, ones_b, sq[:, k, :], k == 0, k == KC - 1)
            act(rs, pm, AF.Ln, bias=EPS, scale=eps_scale)
            act(rs, rs, AF.Exp, scale=-0.5)

        def norm_phase(l, s, w, hT):
            RG.reset()
            xins = [RG.a(16384, F32, "p (k t) -> p k t", k=16) for _ in range(2)]
            sq = RG.a(8192, BF16, "p (k t) -> p k t", k=16)
            rss = [RG.a(1024) for _ in range(2)]
            shj = 0 if w == 0 else 48
            for tt in range(8):
                xin = xins[tt % 2]; rs = rss[tt % 2]
                t0 = s * T + tt * 256
                ld(f"xin{tt % 2}", xin, xT_v[:, :, t0:t0 + 256], dreads=[("xT", s, tt // 2, f) for f in range(16)])
                rms_stats(xin, sq, rs, 256, 1.0 / D)
                vtt(xin, xin, bc_mid(rs, 16), ALU.mult)
                for k in range(KC):
                    if k % 4 != 3:
                        act(hT[:, k, tt * 256:(tt + 1) * 256], xin[:, k, :], AF.Identity,
                            bias=modT[:, l, shj + k, s:s + 1], scale=gsc[:, w, k, s:s + 1])
                    else:
                        vts(hT[:, k, tt * 256:(tt + 1) * 256], xin[:, k, :], gsc[:, w, k, s:s + 1],
                            modT[:, l, shj + k, s:s + 1], ALU.mult, ALU.add)

        def mixing(l, s):
            RH.reset(); RW.reset(); RD.reset(); RM.reset()
            hT = RH.a(65536, BF16, "p (k t) -> p k t", k=16)
            norm_phase(l, s, 0, hT)
            if MIXSTOP == "norm":
                for k in range(KC):
                    dbg_sb(hT[:, k, :], k * 128, 2048)
                raise _Stop()
            ws = [RW.a(12288, BF16, "p (k n) -> p k n", k=16) for _ in range(2)]
            wsi = [0]

            def wload(cols_list):
                i = wsi[0]; wsi[0] ^= 1
                w = ws[i]; o = 0
                for part, (c0, n) in enumerate(cols_list):
                    ldc(f"ws{i}{part}", w[:, :, o:o + n], w_in[l][:, c0:c0 + n].rearrange("(k p) n -> p k n", p=128))
                    o += n
                return w

            def proj_fm(dst, w, c0, scale=None):
                for tt in range(4):
                    pb = big()
                    for kc in range(KC):
                        mm(pb, w[:, kc, c0:c0 + 128], hT[:, kc, tt * 512:(tt + 1) * 512], kc == 0, kc == KC - 1)
                    evac(dst[:, tt * 512:(tt + 1) * 512], pb, scale)

            def proj_tm(dstf, w, c0, n, slotf):
                for kb in range(16):
                    ps = slotf()[:, 0:n]
                    for kc in range(KC):
                        mm(ps, hT[:, kc, kb * 128:(kb + 1) * 128], w[:, kc, c0:c0 + n], kc == 0, kc == KC - 1)
                    dstf(kb, ps)

            qTs = [RD.a(4096, BF16) for _ in range(2)]
            kzs = [[RD.a(4096, BF16) for _ in range(2)] for _ in range(2)]
            Vas = [RD.a(16 * 130 * 2, BF16, "p (j n) -> p j n", j=16) for _ in range(2)]
            pTs = [RD.a(512, BF16) for _ in range(6)]
            odu = [RD.a(512) for _ in range(2)]; od = [RD.a(512) for _ in range(2)]
            odn = [RD.a(256, BF16) for _ in range(2)]
            stg = [RD.a(1024, BF16) for _ in range(2)]
            st4 = [RD.a(64) for _ in range(2)]
            for Va in Vas:
                vmemset(Va[:, :, 128:130], 1.0)
            for par in range(2):
                vmemset(kzs[par][0][64:128, :], 0.0)
                vmemset(kzs[par][1][0:64, :], 0.0)
            pi = [0]
            for h in range(int(os.environ.get('NH', 8))):
                par = h % 2
                w = wload([(h * 128, 128), (1024 + h * 128, 128), (2048 + h * 128, 128)])
                qT, kz, Va = qTs[par], kzs[par], Vas[par]
                proj_fm(qT, w, 0, scale=0.125)
                for tt in range(4):
                    pb = big()
                    for kc in range(KC):
                        mm(pb, w[:, kc, 128:256], hT[:, kc, tt * 512:(tt + 1) * 512], kc == 0, kc == KC - 1)
                    act(kz[0][0:64, tt * 512:(tt + 1) * 512], pb[0:64, :], AF.Copy)
                    vcopy(kz[1][64:128, tt * 512:(tt + 1) * 512], pb[64:128, :])
                proj_tm(lambda kb, ps: evac(Va[:, kb, 0:128], ps), w, 256, 128, small)
                for i in range(16):
                    oacc = [mid() for _ in range(2)]
                    for jg in range(0, i + 1, 2):
                        js = list(range(jg, min(jg + 2, i + 1)))
                        bank = big()
                        for u, j in enumerate(js):
                            for m in range(2):
                                mm(bank[:, u * 256 + m * 128:u * 256 + (m + 1) * 128], kz[m][:, j * 128:(j + 1) * 128],
                                   qT[:, i * 128:(i + 1) * 128], True, True)
                        for u, j in enumerate(js):
                            pT = pTs[pi[0] % 6]; pi[0] += 1
                            c = h * 16 + (i - j)
                            act(pT, bank[:, u * 256:(u + 1) * 256], AF.Exp, bias=btab[:, c:c + 1])
                            if j == i:
                                p3 = pT.rearrange("p (m q) -> p m q", m=2)
                                vtt(p3, p3, bc_mid(tri_b, 2), ALU.mult)
                            for m in range(2):
                                mm(oacc[m][:, 0:129], pT[:, m * 128:(m + 1) * 128], Va[:, j, 0:129], j == 0, j == i)
                    e = i % 2
                    rd = st4[e]
                    vrecip(rd[:, 0:1], oacc[0][:, 128:129]); vrecip(rd[:, 1:2], oacc[1][:, 128:129])
                    vtt(rd[:, 1:2], rd[:, 1:2], lamt[:, 4:5], ALU.mult)
                    vts(odu[e], oacc[0][:, 0:128], rd[:, 0:1], None, ALU.mult, ALU.bypass)
                    vstt(od[e], oacc[1][:, 0:128], rd[:, 1:2], odu[e], ALU.mult, ALU.add)
                    vmemset(rd[:, 2:3], 0.0)
                    act(odu[e], od[e], AF.Square, accum=rd[:, 2:3])
                    act(rd[:, 3:4], rd[:, 2:3], AF.Ln, bias=EPS, scale=1.0 / 128)
                    act(rd[:, 3:4], rd[:, 3:4], AF.Exp, scale=-0.5)
                    vstt(odn[e], od[e], rd[:, 3:4], gsub_bc, ALU.mult, ALU.mult)
                    pt = bf(small())[:, 0:128]
                    tp(pt, odn[e], ident_b)
                    sg = stg[(i // 4) % 2]
                    vcopy(sg[:, (i % 4) * 128:(i % 4 + 1) * 128], pt)
                    if i % 4 == 3:
                        t0 = s * T + (i // 4) * 512
                        stt_(f"stg{(i // 4) % 2}", mix_d[h * 128:(h + 1) * 128, t0:t0 + 512], sg,
                             dwrites=[("mix", s, i // 4, h)])

            RG.reset()
            glow = RG.a(4096, BF16); gqT = RG.a(4096, BF16); gkT = RG.a(4096, BF16)
            gkt = RG.a(4096, BF16, "p (j n) -> p j n", j=16)
            gv = RG.a(8192, BF16, "p (j n) -> p j n", j=16)
            sgg = RG.a(16384, F32, "p (j n) -> p j n", j=16)
            l1 = [RG.a(512) for _ in range(2)]; ebs = [RG.a(512) for _ in range(2)]
            enb = [RG.a(512) for _ in range(2)]; ekd = [RG.a(512) for _ in range(2)]
            qb = [RG.a(256, BF16) for _ in range(2)]; kbb = [RG.a(256, BF16) for _ in range(2)]
            kd = [RG.a(256, BF16) for _ in range(2)]; atm = [RG.a(256, BF16) for _ in range(2)]
            S = RG.a(1024); Sb = [RG.a(512, BF16) for _ in range(2)]
            tg = [RG.a(1024) for _ in range(2)]; ogb = [RG.a(512, BF16) for _ in range(2)]
            junk2 = RM.a(1024)
            stg2 = [RM.a(2048, BF16, "p (r t) -> p r t", r=2) for _ in range(2)]
            st5 = [RM.a(64) for _ in range(2)]
            wl = wload([(6144, 16)])
            for tt in range(4):
                pb = big()
                for kc in range(KC):
                    mm(pb[0:16, :], wl[:, kc, 0:16], hT[:, kc, tt * 512:(tt + 1) * 512], kc == 0, kc == KC - 1)
                evac(glow[0:16, tt * 512:(tt + 1) * 512], pb[0:16, :])
            for hd in range(int(os.environ.get('NG', 4))):
                w1 = wload([(3072 + hd * 128, 128), (3584 + hd * 128, 128)])
                proj_fm(gqT, w1, 0, scale=float(128 ** -0.5))
                proj_fm(gkT, w1, 128)
                proj_tm(lambda kb, ps: evac(gkt[:, kb, :], ps), w1, 128, 128, small)
                w2 = wload([(4096 + hd * 256, 256)])
                proj_tm(lambda kb, ps: evac(gv[:, kb, :], ps), w2, 0, 256, mid)
                w3 = wload([(5120 + hd * 256, 256)])
                proj_tm(lambda kb, ps: act(sgg[:, kb, :], ps, AF.Silu), w3, 0, 256, mid)

                def prep(c):
                    e = c % 2
                    tok = slice(c * 128, (c + 1) * 128)
                    pl = small()
                    mm(pl, glow[0:16, tok], wgk[0:16, hd * 128:(hd + 1) * 128], True, False)
                    mm(pl, ones_b[0:1, 0:128], bgk[0:1, hd * 128:(hd + 1) * 128], False, True)
                    act(l1[e], pl, AF.Exp, scale=-1.0)
                    act(l1[e], l1[e], AF.Ln, bias=1.0)
                    pbt = small(); mm(pbt, l1[e], tri_f, True, True)
                    pu = small(); mm(pu, U_f, l1[e], True, True)
                    act(ebs[e], pbt, AF.Exp, scale=-1.0 / 16)
                    act(enb[e], pbt, AF.Exp, scale=1.0 / 16)
                    act(ekd[e], pu, AF.Exp, scale=-1.0 / 16)
                    vtt(qb[e], gqT[:, tok], ebs[e], ALU.mult)
                    vtt(kbb[e], gkT[:, tok], enb[e], ALU.mult)
                    vtt(kd[e], gkt[:, c, :], ekd[e], ALU.mult)

                def core(c):
                    e = c % 2
                    if c < 15:
                        pds = mid(); mm(pds, kd[e], gv[:, c, :], True, True)
                    pat = small(); mm(pat, kbb[e], qb[e], True, True)
                    vtt(atm[e], pat, tri_f, ALU.mult)
                    po = mid()
                    mm(po, atm[e], gv[:, c, :], True, c == 0)
                    if c > 0:
                        mm(po, qb[e], Sb[(c - 1) % 2], False, True)
                    if c < 15:
                        if c == 0:
                            vcopy(S, pds)
                        else:
                            vstt(S, S, ebs[e][:, 127:128], pds, ALU.mult, ALU.add)
                        act(Sb[c % 2], S, AF.Copy)
                    r5 = st5[e]
                    vmemset(r5[:, 0:1], 0.0)
                    act(junk2, po, AF.Square, accum=r5[:, 0:1])
                    act(r5[:, 1:2], r5[:, 0:1], AF.Ln, bias=EPS, scale=1.0 / 256)
                    act(r5[:, 1:2], r5[:, 1:2], AF.Exp, scale=-0.5)
                    vstt(tg[e], po, r5[:, 1:2], ggla_bc, ALU.mult, ALU.mult)
                    vtt(ogb[e], tg[e], sgg[:, c, :], ALU.mult)
                    sg = stg2[(c // 4) % 2]
                    for r in range(2):
                        pt = bf(small())[:, 0:128]
                        tp(pt, ogb[e][:, r * 128:(r + 1) * 128], ident_b)
                        evac(sg[:, r, (c % 4) * 128:(c % 4 + 1) * 128], pt)
                    if c % 4 == 3:
                        t0 = s * T + (c // 4) * 512
                        for r in range(2):
                            row = 1024 + hd * 256 + r * 128
                            stt_(f"stg2{(c // 4) % 2}{r}", mix_d[row:row + 128, t0:t0 + 512], sg[:, r, :],
                                 dwrites=[("mix", s, c // 4, row // 128)])

                prep(0)
                for c in range(16):
                    if c + 1 < 16:
                        prep(c + 1)
                    core(c)
                if MIXSTOP == f"gla{hd + 1}":
                    dbg_mix(1024, (hd + 1) * 256)
                    raise _Stop()

            RH.reset(); RW.reset(); RM.reset()
            mixa = RH.a(65536, BF16, "p (k t) -> p k t", k=16)
            wos = [RW.a(4096, BF16, "p (k n) -> p k n", k=16) for _ in range(4)]
            xrs = [RM.a(2048) for _ in range(3)]
            for tt in range(4):
                t0 = s * T + tt * 512
                ld(f"mixt{tt}", mixa[:, :, tt * 512:(tt + 1) * 512], mix_v[:, :, t0:t0 + 512],
                   dreads=[("mix", s, tt, r) for r in range(16)])
            n = 0
            for f in range(int(os.environ.get('NFO', 16))):
                wo = wos[f % 4]
                ldc(f"wo{f % 4}", wo, w_o[l][:, f * 128:(f + 1) * 128].rearrange("(k p) n -> p k n", p=128))
                for tt in range(4):
                    t0 = s * T + tt * 512
                    xr = xrs[n % 3]
                    ld(f"xr{n % 3}", xr, xT_d[f * 128:(f + 1) * 128, t0:t0 + 512], dreads=[("xT", s, tt, f)])
                    pb = big()
                    for kc in range(KC):
                        mm(pb, wo[:, kc, :], mixa[:, kc, tt * 512:(tt + 1) * 512], kc == 0, kc == KC - 1)
                    vstt(xr, pb, modT[:, l, 32 + f, s:s + 1], xr, ALU.mult, ALU.add)
                    stt_(f"xs{n % 3}", xT_d[f * 128:(f + 1) * 128, t0:t0 + 512], xr, dwrites=[("xT", s, tt, f)])
                    n += 1

        def ffn(l, s):
            RH.reset(); RW.reset(); RD.reset(); RM.reset()
            hT = RH.a(65536, BF16, "p (k t) -> p k t", k=16)
            norm_phase(l, s, 1, hT)
            wus = [RW.a(8192, BF16, "p (k n) -> p k n", k=16) for _ in range(2)]
            defer_mod = (l == 0 and s == 0 and not os.environ.get("BENCH"))
            if defer_mod:
                RG.reset()
                was1 = [RG.a(16384, BF16, "p (k n) -> p k n", k=16) for _ in range(3)]
                mrow1 = RM.a(2048); brows1 = [RM.a(2048) for _ in range(2)]
            gbs = [RD.a(514 * 4) for _ in range(2)]; t1s = [RD.a(2048) for _ in range(2)]
            t2s = [RD.a(2048) for _ in range(2)]; accs = [RD.a(4096, BF16) for _ in range(2)]
            n = 0
            for j in range(int(os.environ.get('NJ', JC))):
                wu = wus[j % 2]
                ldc(f"wu{j % 2}0", wu[:, :, 0:128], w_up[l][:, j * 128:(j + 1) * 128].rearrange("(k p) n -> p k n", p=128))
                ldc(f"wu{j % 2}1", wu[:, :, 128:256], w_up[l][:, DFF + j * 128:DFF + (j + 1) * 128].rearrange("(k p) n -> p k n", p=128))
                acc = accs[j % 2]
                for tt in range(4):
                    pu = big()
                    for kc in range(KC):
                        mm(pu, wu[:, kc, 0:128], hT[:, kc, tt * 512:(tt + 1) * 512], kc == 0, kc == KC - 1)
                    pg = big()
                    for kc in range(KC):
                        mm(pg, wu[:, kc, 128:256], hT[:, kc, tt * 512:(tt + 1) * 512], kc == 0, kc == KC - 1)
                    gb = gbs[n % 2]; t1 = t1s[n % 2]; t2 = t2s[n % 2]
                    if tt == 0:
                        vmemset(gb[:, 0:2], 0.0)
                    else:
                        vcopy(gb[:, 0:2], gbs[(n - 1) % 2][:, 512:514])
                    act(gb[:, 2:514], pg, AF.Copy)
                    vts(t1, gb[:, 2:514], wcv[:, 2, j:j + 1], bcv[:, j:j + 1], ALU.mult, ALU.add)
                    vstt(t1, gb[:, 1:513], wcv[:, 1, j:j + 1], t1, ALU.mult, ALU.add)
                    vstt(t1, gb[:, 0:512], wcv[:, 0, j:j + 1], t1, ALU.mult, ALU.add)
                    act(t2, t1, AF.Gelu)
                    vtt(acc[:, tt * 512:(tt + 1) * 512], t2, pu, ALU.mult)
                    n += 1
                stt_(f"acc{j % 2}", act_d[j * 128:(j + 1) * 128, s * T:(s + 1) * T], acc, dwrites=[("act", s, j)])
                if defer_mod and j < 24:
                    mod_group(1, j, was1[j % 3], f"wb{j % 3}", brows1[j % 2], f"brow{j % 2}", mrow1)
            RHW.reset(); RD.reset(); RM.reset()
            at = RHW.a(JC * 1024 * 2, BF16, "p (j t) -> p j t", j=JC)
            wds = [RD.a(JC * 128 * 2, BF16, "p (j n) -> p j n", j=JC) for _ in range(3)]
            xrs = [RM.a(2048) for _ in range(3)]
            n = 0
            for half in range(2):
                for q in range(2):
                    t0 = s * T + half * 1024 + q * 512
                    ld(f"at{q}", at[:, :, q * 512:(q + 1) * 512], act_v[:, :, t0:t0 + 512],
                       dreads=[("act", s, j) for j in range(JC)])
                for f in range(int(os.environ.get('NF', 16))):
                    wd = wds[f % 3]
                    ldc(f"wd{f % 3}", wd, w_dn[l][:, f * 128:(f + 1) * 128].rearrange("(j p) n -> p j n", p=128))
                    for q in range(2):
                        tt = half * 2 + q
                        t0 = s * T + tt * 512
                        xr = xrs[n % 3]
                        ld(f"xr{n % 3}", xr, xT_d[f * 128:(f + 1) * 128, t0:t0 + 512], dreads=[("xT", s, tt, f)])
                        pb = big()
                        for j in range(JC):
                            mm(pb, wd[:, j, :], at[:, j, q * 512:(q + 1) * 512], j == 0, j == JC - 1)
                        vstt(xr, pb, modT[:, l, 80 + f, s:s + 1], xr, ALU.mult, ALU.add)
                        stt_(f"xs{n % 3}", xT_d[f * 128:(f + 1) * 128, t0:t0 + 512], xr, dwrites=[("xT", s, tt, f)])
                        n += 1

        def final():
            RG.reset(); RD.reset()
            xins = [RG.a(16384, F32, "p (k t) -> p k t", k=16) for _ in range(2)]
            sq = RG.a(8192, BF16, "p (k t) -> p k t", k=16)
            rss = [RG.a(1024) for _ in range(2)]
            ystg = [RD.a(8192) for _ in range(2)]
            n = 0
            for s in range(NSQ):
                for tt in range(8):
                    xin = xins[tt % 2]; rs = rss[tt % 2]
                    t0 = s * T + tt * 256
                    ld(f"xin{tt % 2}", xin, xT_v[:, :, t0:t0 + 256], dreads=[("xT", s, tt // 2, f) for f in range(16)])
                    rms_stats(xin, sq, rs, 256, 1.0 / D)
                    vtt(xin, xin, bc_mid(rs, 16), ALU.mult)
                    for k in range(KC):
                        act(xin[:, k, :], xin[:, k, :], AF.Copy, scale=gfin[:, k:k + 1])
                    for sub in range(2):
                        ys = ystg[n % 2]
                        for g in range(4):
                            pb = big()
                            for q in range(4):
                                k = g * 4 + q
                                tp(pb[:, q * 128:(q + 1) * 128], xin[:, k, sub * 128:(sub + 1) * 128], ident_f)
                            evac(ys[:, g * 512:(g + 1) * 512], pb)
                        r0 = t0 + sub * 128
                        stt_(f"ystg{n % 2}", y_d[r0:r0 + 128, :], ys)
                        n += 1

        stages = []
        for l in range(2):
            stages.append(("setup", l, None))
            for s in range(NSQ):
                stages.append(("mix", l, s))
            for s in range(NSQ):
                stages.append(("ffn", l, s))
        done = False
        if os.environ.get("BENCH"):
            pass
        elif stop_after == ("conv", 0, None):
            for blk in range(NTOK // 128):
                conv_block(blk)
            stages = []; done = True
        else:
            mod_phase(True)
        if stop_after == ("mod", 0, None):
            stages = []; done = True
        if os.environ.get("BENCH") == "ffn":
            stages = [("setup", 0, None), ("ffn", 0, 0)]
        if os.environ.get("BENCH") == "mix":
            stages = [("setup", 0, None), ("mix", 0, 0)]
        try:
            for (kind, l, s) in stages:
                if kind == "setup":
                    layer_setup(l)
                elif kind == "mix":
                    mixing(l, s)
                else:
                    ffn(l, s)
                if stop_after == (kind, l, s):
                    done = True
                    break
            if not done:
                final()
            if dbg_d is not None:
                dump_and_finish()
        except _Stop:
            pass
        P.wait_all("sp")
        counts = P.emit(); counts["ntrk"] = len(P.cnt)
    return nc, counts


def _consts():
    p = np.arange(128)
    ident = np.eye(128, dtype=np.float32)
    tri = (p[:, None] <= p[None, :]).astype(np.float32)
    U = (p[:, None] > p[None, :]).astype(np.float32)
    slopes = np.array([(2.0 ** (-8.0 / 8)) ** (i + 1) for i in range(8)], dtype=np.float64)
    bt = np.zeros((128, 128), np.float32)
    for h in range(8):
        for d in range(16):
            bt[:, h * 16 + d] = slopes[h] * (p - 64 - 128 * d)
    return np.concatenate([ident, tri, U, bt], axis=1).astype(np.float32)


_CACHE = {}


def kernel(**inputs):
    stop_after = inputs.pop("_stop_after", None)
    key = ("nc", stop_after)
    if key not in _CACHE:
        _CACHE[key] = build_program(stop_after)
    nc, counts = _CACHE[key]
    f = lambda a: np.ascontiguousarray(np.asarray(a, dtype=np.float32))
    shared = {k: f(inputs[k]) for k in ("w_ada", "b_ada", "g_mix_norm", "w_in", "g_diff_subln", "w_gk_up", "b_gk",
                                        "g_gla_norm", "w_o", "g_ffn_norm", "w_up", "w_conv", "b_conv", "w_down",
                                        "g_final")}
    shared["diff_lambda"] = f(inputs["diff_lambda"]).reshape(2, 256)
    shared["consts"] = _consts()
    x = f(inputs["x"]); c = f(inputs["c"])
    ncores = int(inputs.pop("_ncores", 8)) if "_ncores" in inputs else 8
    in_maps = []
    for i in range(ncores):
        m = dict(shared)
        m["x"] = x[2 * i:2 * i + 2].reshape(NTOK, D)
        m["c"] = c[2 * i:2 * i + 2]
        in_maps.append(m)
    res = run_bass_kernel_spmd(nc, in_maps, core_ids=list(range(ncores)))
    if stop_after is not None:
        return [r["dbg"] for r in res.results]
    out = np.stack([r["y"].reshape(NSQ, T, D) for r in res.results], axis=0).reshape(ncores * NSQ, T, D)
    return out.astype(np.float32)
```

```python
import numpy as np
import concourse.bass as bass
import concourse.mybir as mybir

F32 = mybir.dt.float32
BF16 = mybir.dt.bfloat16
AF = mybir.ActivationFunctionType
ALU = mybir.AluOpType
AX = mybir.AxisListType

CELL = 64
_DS = {F32: 4, BF16: 2}


def _dsize(dt):
    return _DS.get(dt, 4)


class Prog:
    ENG = ("pe", "act", "dve", "pool", "sp")

    def __init__(self, nc, sb_bytes, same_engine_sync=True):
        self.nc = nc
        self.ops = {e: [] for e in self.ENG}
        self.trk = {}
        self.cnt = []
        self.sig = []
        self.is_chan = []
        for e in ("pe", "act", "dve", "pool"):
            self._tracker(e, False)
        self.ncell = {"sb": sb_bytes // CELL, "ps": 16384 // CELL}
        self.MAXT = 120
        self.lw = {k: np.zeros((self.MAXT, n), np.int32) for k, n in self.ncell.items()}
        self.lr = {k: np.zeros((self.MAXT, n), np.int32) for k, n in self.ncell.items()}
        self.dram = {}
        self.bank_last = np.zeros((self.MAXT, 8), np.int32)
        self.same = same_engine_sync
        self.waited = {e: {} for e in self.ENG}

    def _tracker(self, name, is_chan):
        if name not in self.trk:
            self.trk[name] = len(self.cnt)
            self.cnt.append(0)
            self.sig.append(set())
            self.is_chan.append(is_chan)
        return self.trk[name]

    @staticmethod
    def cells(ap):
        ds = _dsize(ap.dtype)
        dims = ap.ap
        pstep = dims[0][0]
        off = (int(ap.offset) % pstep) * ds if pstep > 0 else int(ap.offset) * ds
        space = "ps" if "PSum" in type(ap.tensor).__name__ else "sb"
        starts = np.array([off], np.int64)
        inner = ds
        fd = [(s, c) for (s, c) in dims[1:] if c > 1 and s != 0]
        fd.sort(key=lambda x: x[0])
        rest = []
        for s, c in fd:
            if s * ds == inner:
                inner *= c
            else:
                rest.append((s, c))
        for s, c in rest:
            starts = (starts[:, None] + (np.arange(c, dtype=np.int64) * s * ds)[None, :]).ravel()
        lo = starts // CELL
        hi = (starts + inner - 1) // CELL
        n = int((hi - lo).max()) + 1
        cs = (lo[:, None] + np.arange(n)[None, :])
        cs = np.minimum(cs, hi[:, None]).ravel()
        return space, np.unique(cs)

    def _deps(self, reads, writes, dreads, dwrites, me):
        deps = np.zeros(self.MAXT, np.int32)
        acc = []
        bdeps = np.zeros(self.MAXT, np.int32)
        for ap in reads:
            sp, cs = self.cells(ap)
            acc.append((sp, cs, False))
            np.maximum(deps, self.lw[sp][:, cs].max(axis=1), out=deps)
            if sp == "ps":
                np.maximum(bdeps, self.bank_last[:, np.unique(cs // (2048 // CELL))].max(axis=1), out=bdeps)
        for ap in writes:
            sp, cs = self.cells(ap)
            acc.append((sp, cs, True))
            np.maximum(deps, self.lw[sp][:, cs].max(axis=1), out=deps)
            np.maximum(deps, self.lr[sp][:, cs].max(axis=1), out=deps)
            if sp == "ps":
                np.maximum(bdeps, self.bank_last[:, np.unique(cs // (2048 // CELL))].max(axis=1), out=bdeps)
        bdeps[me] = 0
        np.maximum(deps, bdeps, out=deps)
        d = {int(t): int(deps[t]) for t in np.nonzero(deps)[0]}
        for k in dreads:
            rec = self.dram.setdefault(k, ({}, {}))
            for t, i in rec[0].items():
                d[t] = max(d.get(t, 0), i)
        for k in dwrites:
            rec = self.dram.setdefault(k, ({}, {}))
            for t, i in list(rec[0].items()) + list(rec[1].items()):
                d[t] = max(d.get(t, 0), i)
        return d, acc

    def _commit(self, t, idx, acc, dreads, dwrites):
        for sp, cs, w in acc:
            (self.lw if w else self.lr)[sp][t, cs] = idx
            if sp == "ps":
                self.bank_last[t, np.unique(cs // (2048 // CELL))] = idx
        for k in dreads:
            self.dram[k][1][t] = idx
        for k in dwrites:
            self.dram[k][0][t] = idx
            self.dram[k][1].clear()

    def op(self, eng, fn, reads=(), writes=(), dreads=(), dwrites=()):
        t = self.trk[eng]
        d, acc = self._deps(reads, writes, dreads, dwrites, t)
        if eng == "pe" or not self.same:
            d.pop(t, None)
        self.cnt[t] += 1
        idx = self.cnt[t]
        waits = self._waits(eng, d)
        self.ops[eng].append((fn, waits, t, idx))
        self._commit(t, idx, acc, dreads, dwrites)

    def dma(self, queue, chan, fn, reads=(), writes=(), dreads=(), dwrites=()):
        t = self._tracker("ch:" + chan, True)
        d, acc = self._deps(reads, writes, dreads, dwrites, t)
        d.pop(t, None)
        self.cnt[t] += 1
        idx = self.cnt[t]
        waits = self._waits(queue, d)
        self.ops[queue].append((fn, waits, t, idx))
        self._commit(t, idx, acc, dreads, dwrites)

    def _waits(self, eng, d):
        w = []
        wd = self.waited[eng]
        for t, i in d.items():
            if wd.get(t, 0) >= i:
                continue
            wd[t] = i
            self.sig[t].add(i)
            w.append((t, i))
        return w

    def wait_all(self, eng):
        d = {t: c for t, c in enumerate(self.cnt) if c > 0}
        d.pop(self.trk.get(eng, -1), None)
        waits = self._waits(eng, d)
        self.ops[eng].append((None, waits, None, None))

    def emit(self):
        nc = self.nc
        nt = len(self.cnt)
        rank = []
        for t in range(nt):
            s = sorted(self.sig[t])
            rank.append({i: k + 1 for k, i in enumerate(s)})
        import contextlib
        with contextlib.ExitStack() as st:
            sems = [st.enter_context(nc.semaphore(f"s{t}")) for t in range(nt)]
            block = st.enter_context(nc.Block())

            def run(eng_name, e):
                for fn, waits, t, idx in self.ops[eng_name]:
                    for (wt, wi) in waits:
                        v = 16 * wi if self.is_chan[wt] else rank[wt][wi]
                        e.wait_ge(sems[wt], v)
                    if fn is None:
                        continue
                    ins = fn(e)
                    if self.is_chan[t]:
                        ins.then_inc(sems[t], 16)
                    elif idx in rank[t]:
                        ins.then_inc(sems[t], 1)

            @block.tensor
            def _(e):
                run("pe", e)

            @block.scalar
            def _(e):
                run("act", e)

            @block.vector
            def _(e):
                run("dve", e)

            @block.gpsimd
            def _(e):
                run("pool", e)

            @block.sync
            def _(e):
                run("sp", e)
        return {e: len(v) for e, v in self.ops.items()}


import math, os
from concourse.bass_utils import run_bass_kernel_spmd

D = 2048; T = 2048; NSQ = 2; NTOK = NSQ * T; KC = 16; DFF = 5632; JC = 44
NPROJ = 6160; EPS = 1e-6
KB_ = 1024


def _lam_init(l):
    return 0.8 - 0.6 * math.exp(-0.3 * l)


def build_program(stop_after=None):
    nc = bass.Bass("TRN2", target_bir_lowering=False)
    dt_in = lambda n, s: nc.dram_tensor(n, s, F32, kind="ExternalInput").ap()
    x_d = dt_in("x", [NTOK, D]); c_d = dt_in("c", [NSQ, D])
    w_ada = dt_in("w_ada", [2, D, 6 * D]); b_ada = dt_in("b_ada", [2, 6 * D])
    g_mix = dt_in("g_mix_norm", [2, D]); w_in = dt_in("w_in", [2, D, NPROJ])
    dlam = dt_in("diff_lambda", [2, 256]); g_sub = dt_in("g_diff_subln", [2, 128])
    w_gk = dt_in("w_gk_up", [2, 16, 512]); b_gk = dt_in("b_gk", [2, 512])
    g_gla = dt_in("g_gla_norm", [2, 256]); w_o = dt_in("w_o", [2, D, D])
    g_ffn = dt_in("g_ffn_norm", [2, D]); w_up = dt_in("w_up", [2, D, 2 * DFF])
    w_cv = dt_in("w_conv", [2, 3, DFF]); b_cv = dt_in("b_conv", [2, DFF])
    w_dn = dt_in("w_down", [2, DFF, D]); g_fin = dt_in("g_final", [D])
    cst_d = dt_in("consts", [128, 512])
    y_d = nc.dram_tensor("y", [NTOK, D], F32, kind="ExternalOutput").ap()
    xT_d = nc.dram_tensor("xT_s", [D, NTOK], F32, kind="Internal").ap()
    mix_d = nc.dram_tensor("mix_s", [D, NTOK], BF16, kind="Internal").ap()
    act_d = nc.dram_tensor("act_s", [DFF, NTOK], BF16, kind="Internal").ap()
    dbg_d = None
    if stop_after is not None:
        dbg_d = nc.dram_tensor("dbg", [D, NTOK], F32, kind="ExternalOutput").ap()
    xT_v = xT_d.rearrange("(k p) t -> p k t", p=128)
    mix_v = mix_d.rearrange("(k p) t -> p k t", p=128)
    act_v = act_d.rearrange("(j p) t -> p j t", p=128)

    SB = 204 * KB_
    import contextlib
    with contextlib.ExitStack() as st:
        arena = st.enter_context(nc.sbuf_tensor("arena", [128, SB // 4], F32))
        psum = st.enter_context(nc.psum_tensor("psum", [128, 4096], F32))
        P = Prog(nc, SB)

        class Region:
            def __init__(s, lo, hi):
                s.lo, s.hi, s.o = lo, hi, lo

            def reset(s):
                s.o = s.lo

            def a(s, nbytes, dt=F32, pat=None, **kw):
                n = (nbytes + 63) // 64 * 64
                assert s.o + n <= s.hi, ("region overflow", s.lo, s.hi, s.o, n)
                v = arena[:, s.o // 4:(s.o + nbytes + 3) // 4]
                s.o += n
                if dt == BF16:
                    v = v.bitcast(BF16)[:, 0:nbytes // 2]
                else:
                    v = v[:, 0:nbytes // 4]
                if pat:
                    v = v.rearrange(pat, **kw)
                return v

        RC = Region(0, 12 * KB_); RH = Region(12 * KB_, 76 * KB_); RW = Region(76 * KB_, 100 * KB_)
        RHW = Region(12 * KB_, 100 * KB_)
        RD = Region(100 * KB_, 144 * KB_); RG = Region(144 * KB_, 196 * KB_); RM = Region(196 * KB_, 204 * KB_)

        _rr = {"big": 0, "small": 0, "mid": 0}

        def big():
            i = _rr["big"]; _rr["big"] = (i + 1) % 4
            return psum[:, i * 512:(i + 1) * 512]

        def small():
            i = _rr["big"]; _rr["big"] = (i + 1) % 4
            return psum[:, i * 512:i * 512 + 128]

        def mid():
            i = _rr["mid"]; _rr["mid"] = (i + 1) % 4
            return psum[:, 2048 + i * 512:2048 + i * 512 + 256]

        def bf(ps):
            return ps.bitcast(BF16)

        def mm(out, lhsT, rhs, start, stop):
            P.op("pe", lambda e: e.matmul(out, lhsT=lhsT, rhs=rhs, start=start, stop=stop),
                 reads=[lhsT, rhs], writes=[out])

        def tp(out, in_, ident):
            P.op("pe", lambda e: e.transpose(out=out, in_=in_, identity=ident), reads=[in_, ident], writes=[out])

        def act(out, in_, func, bias=None, scale=None, accum=None):
            rd = [in_] + [a for a in (bias, scale) if a is not None and not isinstance(a, float)]
            wr = [out] + ([accum] if accum is not None else [])
            kw = {}
            if bias is not None: kw["bias"] = bias
            if scale is not None: kw["scale"] = scale
            if accum is not None: kw["accum_out"] = accum
            P.op("act", lambda e: e.activation(out=out, in_=in_, func=func, **kw), reads=rd, writes=wr)

        def vcopy(out, in_, eng="dve"):
            P.op(eng, lambda e: e.tensor_copy(out=out, in_=in_), reads=[in_], writes=[out])

        def vtt(out, a, b, op, eng="dve"):
            P.op(eng, lambda e: e.tensor_tensor(out=out, in0=a, in1=b, op=op), reads=[a, b], writes=[out])

        def vts(out, a, s1, s2, op0, op1, eng="dve"):
            rd = [a] + [s for s in (s1, s2) if s is not None and not isinstance(s, float)]
            P.op(eng, lambda e: e.tensor_scalar(out=out, in0=a, scalar1=s1, scalar2=s2, op0=op0, op1=op1),
                 reads=rd, writes=[out])

        def vstt(out, a, s, b, op0, op1, eng="dve"):
            rd = [a, b] + ([s] if not isinstance(s, float) else [])
            P.op(eng, lambda e: e.scalar_tensor_tensor(out=out, in0=a, scalar=s, in1=b, op0=op0, op1=op1),
                 reads=rd, writes=[out])

        def vrecip(out, in_):
            P.op("dve", lambda e: e.reciprocal(out=out, in_=in_), reads=[in_], writes=[out])

        def vmemset(ap, val, eng="dve"):
            P.op(eng, lambda e: e.memset(ap, val), writes=[ap])

        def ld(chan, out, in_, dreads=(), slow=False):
            if slow:
                P.dma("sp", chan, lambda e: e.dma_start(out=out, in_=in_, allow_slow_non_contiguous=True),
                      writes=[out], dreads=dreads)
            else:
                P.dma("sp", chan, lambda e: e.dma_start(out=out, in_=in_), writes=[out], dreads=dreads)

        def ldc(chan, out, in_):
            P.dma("pool", chan, lambda e: e.dma_start(out=out, in_=in_), writes=[out])

        def stt_(chan, out, in_, dwrites=()):
            P.dma("sp", chan, lambda e: e.dma_start(out=out, in_=in_), reads=[in_], dwrites=dwrites)

        evac_rr = [0]

        def evac(out, in_, scale=None):
            evac_rr[0] ^= 1
            if scale is not None:
                act(out, in_, AF.Copy, scale=scale)
            elif evac_rr[0]:
                act(out, in_, AF.Copy)
            else:
                vcopy(out, in_)

        def bc_mid(ap, n):
            return bass.AP(ap.tensor, ap.offset, [list(ap.ap[0]), [0, n], list(ap.ap[1])])

        cst = RC.a(2048, F32)
        ident_f = cst[:, 0:128]; tri_f = cst[:, 128:256]; U_f = cst[:, 256:384]; btab = cst[:, 384:512]
        ident_b = RC.a(256, BF16); tri_b = RC.a(256, BF16); ones_b = RC.a(256, BF16)
        modT = RC.a(2 * 96 * 2 * 4, F32, "p (l j b) -> p l j b", l=2, j=96)
        gmix = RC.a(64); gffn = RC.a(64); gfin = RC.a(64)
        gsc = RC.a(2 * 16 * 2 * 4, F32, "p (w k b) -> p w k b", w=2, k=16)
        wcv = RC.a(3 * JC * 4, F32, "p (j k) -> p j k", j=3); bcv = RC.a(JC * 4)
        gsub_bc = RC.a(512); ggla_bc = RC.a(1024); dl = RC.a(1024)
        lamt = RC.a(64)
        wgk = RC.a(1024, BF16); bgk = RC.a(1024, BF16)
        cf = RC.a(128, F32, "p (k b) -> p k b", k=16); cb = RC.a(64, BF16, "p (k b) -> p k b", k=16)
        stat = RC.a(512)

        ld("cst", cst, cst_d)
        vcopy(ident_b, ident_f); vcopy(tri_b, tri_f); vmemset(ones_b, 1.0)
        ld("gfin", gfin, g_fin.rearrange("(k p) -> p k", p=128), slow=True)

        RG.reset()
        xtok = [RG.a(8192) for _ in range(2)]
        cstg = [RG.a(8192, F32, "p (k t) -> p k t", k=16) for _ in range(2)]

        def conv_block(blk):
            s, t5 = blk // 16, (blk % 16) // 4
            xt = xtok[blk % 2]; cs = cstg[blk % 2]
            ld(f"xtok{blk % 2}", xt, x_d[blk * 128:(blk + 1) * 128, :])
            for g in range(4):
                pb = big()
                for q in range(4):
                    k = g * 4 + q
                    tp(pb[:, q * 128:(q + 1) * 128], xt[:, k * 128:(k + 1) * 128], ident_f)
                evac(cs[:, g * 4:(g + 1) * 4, :], pb.rearrange("p (k t) -> p k t", k=4))
            stt_(f"cstg{blk % 2}", xT_v[:, :, blk * 128:(blk + 1) * 128], cs,
                 dwrites=[("xT", s, t5, f) for f in range(16)])

        class _Stop(Exception):
            pass

        def dump_and_finish():
            P.wait_all("sp")
            for r in range(16):
                P.dma("sp", f"dbg{r % 4}", lambda e, r=r: e.dma_start(out=dbg_d[r * 128:(r + 1) * 128, :], in_=xT_d[r * 128:(r + 1) * 128, :]))

        MIXSTOP = os.environ.get("MIXSTOP", "")

        def dbg_sb(src, row0, ncol):
            RM.reset()
            stg_ = RM.a(8192)
            for c0 in range(0, ncol, 2048):
                n_ = min(2048, ncol - c0)
                vcopy(stg_[:, 0:n_], src[:, c0:c0 + n_])
                stt_("dbgsb", dbg_d[row0:row0 + 128, c0:c0 + n_], stg_[:, 0:n_])

        def dbg_mix(row0, nrows):
            RW.reset()
            b16 = RW.a(4096, BF16)
            for r in range(row0, row0 + nrows, 128):
                P.wait_all("sp")
                ld("dbgm", b16, mix_d[r:r + 128, 0:2048])
                dbg_sb(b16, r, 2048)

        def mod_phase(with_conv):
            for b in range(NSQ):
                ld(f"cf{b}", cf[:, :, b], c_d[b].rearrange("(k p) -> p k", p=128), slow=True)
            act(cb, cf, AF.Silu)
            RH.reset(); RM.reset()
            was = [RH.a(16384, BF16, "p (k n) -> p k n", k=16) for _ in range(3)]
            mrow = RM.a(2048)
            brows = [RM.a(2048) for _ in range(2)]
            blk = 0
            for l in range(2):
                for g in range(24):
                    n = l * 24 + g
                    wa = was[n % 3]; brow = brows[n % 2]
                    ldc(f"wa{n % 3}", wa, w_ada[l][:, g * 512:(g + 1) * 512].rearrange("(k p) n -> p k n", p=128))
                    ld(f"brow{n % 2}", brow[0:2, :], b_ada[l, g * 512:(g + 1) * 512].partition_broadcast(2))
                    if with_conv:
                        while blk < NTOK // 128 and blk * 48 <= n * 32:
                            conv_block(blk); blk += 1
                    pb = big()
                    for kc in range(KC):
                        mm(pb[0:2, :], cb[:, kc, :], wa[:, kc, :], kc == 0, kc == KC - 1)
                    vtt(mrow[0:2, :], pb[0:2, :], brow[0:2, :], ALU.add)
                    pss = small()
                    for q in range(4):
                        mm(pss[:, q * 2:(q + 1) * 2], mrow[0:2, q * 128:(q + 1) * 128], ident_f[0:2, 0:2], True, True)
                    vcopy(modT[:, l, g * 4:(g + 1) * 4, :], pss[:, 0:8].rearrange("p (q b) -> p q b", q=4))
            if with_conv:
                while blk < NTOK // 128:
                    conv_block(blk); blk += 1

        def layer_setup(l):
            ld("gmix", gmix, g_mix[l].rearrange("(k p) -> p k", p=128), slow=True)
            ld("gffn", gffn, g_ffn[l].rearrange("(k p) -> p k", p=128), slow=True)
            for w, (gv_, j0) in enumerate(((gmix, 16), (gffn, 64))):
                for b in range(NSQ):
                    vts(gsc[:, w, :, b], modT[:, l, j0:j0 + 16, b], 1.0, None, ALU.add, ALU.bypass)
                    vtt(gsc[:, w, :, b], gsc[:, w, :, b], gv_, ALU.mult)
            for j in range(3):
                ld(f"wcv{j}", wcv[:, j, :], w_cv[l, j].rearrange("(k p) -> p k", p=128), slow=True)
            ld("bcv", bcv, b_cv[l].rearrange("(k p) -> p k", p=128), slow=True)
            ld("gsub", gsub_bc, g_sub[l].partition_broadcast(128))
            vts(gsub_bc, gsub_bc, float(1.0 - _lam_init(l)), None, ALU.mult, ALU.bypass)
            ld("ggla", ggla_bc, g_gla[l].partition_broadcast(128))
            ld("dl", dl, dlam[l].partition_broadcast(128))
            dl4 = dl.rearrange("p (a w d) -> p a w d", a=2, w=2)
            prod = stat[:, 0:128].rearrange("p (a d) -> p a d", a=2)
            vtt(prod, dl4[:, :, 0, :], dl4[:, :, 1, :], ALU.mult)
            P.op("dve", lambda e: e.reduce_sum(out=lamt[:, 0:2], in_=prod, axis=AX.X), reads=[prod], writes=[lamt[:, 0:2]])
            act(lamt[:, 2:4], lamt[:, 0:2], AF.Exp)
            vtt(lamt[:, 4:5], lamt[:, 3:4], lamt[:, 2:3], ALU.subtract)
            vts(lamt[:, 4:5], lamt[:, 4:5], float(-_lam_init(l)), None, ALU.add, ALU.bypass)
            ldc("wgk", wgk[0:16, :], w_gk[l])
            ldc("bgk", bgk[0:1, :], b_gk[l:l + 1, :])

        def rms_stats(xin, sq, rs, ntok, eps_scale):
            act(sq.rearrange("p k t -> p (k t)"), xin.rearrange("p k t -> p (k t)"), AF.Square)
            pm = mid()[:, 0:ntok]
            for k in range(KC):
                mm(pm, ones_b, sq[:, k, :], k == 0, k == KC - 1)
            act(rs, pm, AF.Ln, bias=EPS, scale=eps_scale)
            act(rs, rs, AF.Exp, scale=-0.5)

        def norm_phase(l, s, w, hT):
            RG.reset()
            xins = [RG.a(16384, F32, "p (k t) -> p k t", k=16) for _ in range(2)]
            sq = RG.a(8192, BF16, "p (k t) -> p k t", k=16)
            rss = [RG.a(1024) for _ in range(2)]
            shj = 0 if w == 0 else 48
            for tt in range(8):
                xin = xins[tt % 2]; rs = rss[tt % 2]
                t0 = s * T + tt * 256
                ld(f"xin{tt % 2}", xin, xT_v[:, :, t0:t0 + 256], dreads=[("xT", s, tt // 2, f) for f in range(16)])
                rms_stats(xin, sq, rs, 256, 1.0 / D)
                vtt(xin, xin, bc_mid(rs, 16), ALU.mult)
                for k in range(KC):
                    vts(hT[:, k, tt * 256:(tt + 1) * 256], xin[:, k, :], gsc[:, w, k, s:s + 1],
                        modT[:, l, shj + k, s:s + 1], ALU.mult, ALU.add)

        def mixing(l, s):
            RH.reset(); RW.reset(); RD.reset(); RM.reset()
            hT = RH.a(65536, BF16, "p (k t) -> p k t", k=16)
            norm_phase(l, s, 0, hT)
            if MIXSTOP == "norm":
                for k in range(KC):
                    dbg_sb(hT[:, k, :], k * 128, 2048)
                raise _Stop()
            ws = [RW.a(12288, BF16, "p (k n) -> p k n", k=16) for _ in range(2)]
            wsi = [0]

            def wload(cols_list):
                i = wsi[0]; wsi[0] ^= 1
                w = ws[i]; o = 0
                for part, (c0, n) in enumerate(cols_list):
                    ldc(f"ws{i}{part}", w[:, :, o:o + n], w_in[l][:, c0:c0 + n].rearrange("(k p) n -> p k n", p=128))
                    o += n
                return w

            def proj_fm(dst, w, c0, scale=None):
                for tt in range(4):
                    pb = big()
                    for kc in range(KC):
                        mm(pb, w[:, kc, c0:c0 + 128], hT[:, kc, tt * 512:(tt + 1) * 512], kc == 0, kc == KC - 1)
                    evac(dst[:, tt * 512:(tt + 1) * 512], pb, scale)

            def proj_tm(dstf, w, c0, n, slotf):
                for kb in range(16):
                    ps = slotf()[:, 0:n]
                    for kc in range(KC):
                        mm(ps, hT[:, kc, kb * 128:(kb + 1) * 128], w[:, kc, c0:c0 + n], kc == 0, kc == KC - 1)
                    dstf(kb, ps)

            qTs = [RD.a(4096, BF16) for _ in range(2)]
            kzs = [[RD.a(4096, BF16) for _ in range(2)] for _ in range(2)]
            Vas = [RD.a(16 * 130 * 2, BF16, "p (j n) -> p j n", j=16) for _ in range(2)]
            pTs = [RD.a(512, BF16) for _ in range(6)]
            odu = [RD.a(512) for _ in range(2)]; od = [RD.a(512) for _ in range(2)]
            odn = [RD.a(256, BF16) for _ in range(2)]
            stg = [RD.a(1024, BF16) for _ in range(2)]
            st4 = [RD.a(64) for _ in range(2)]
            for Va in Vas:
                vmemset(Va[:, :, 128:130], 1.0)
            for par in range(2):
                vmemset(kzs[par][0][64:128, :], 0.0)
                vmemset(kzs[par][1][0:64, :], 0.0)
            pi = [0]
            for h in range(8):
                par = h % 2
                w = wload([(h * 128, 128), (1024 + h * 128, 128), (2048 + h * 128, 128)])
                qT, kz, Va = qTs[par], kzs[par], Vas[par]
                proj_fm(qT, w, 0, scale=0.125)
                for tt in range(4):
                    pb = big()
                    for kc in range(KC):
                        mm(pb, w[:, kc, 128:256], hT[:, kc, tt * 512:(tt + 1) * 512], kc == 0, kc == KC - 1)
                    act(kz[0][0:64, tt * 512:(tt + 1) * 512], pb[0:64, :], AF.Copy)
                    vcopy(kz[1][64:128, tt * 512:(tt + 1) * 512], pb[64:128, :])
                proj_tm(lambda kb, ps: evac(Va[:, kb, 0:128], ps), w, 256, 128, small)
                for i in range(16):
                    oacc = [mid() for _ in range(2)]
                    for jg in range(0, i + 1, 2):
                        js = list(range(jg, min(jg + 2, i + 1)))
                        bank = big()
                        for u, j in enumerate(js):
                            for m in range(2):
                                mm(bank[:, u * 256 + m * 128:u * 256 + (m + 1) * 128], kz[m][:, j * 128:(j + 1) * 128],
                                   qT[:, i * 128:(i + 1) * 128], True, True)
                        for u, j in enumerate(js):
                            pT = pTs[pi[0] % 6]; pi[0] += 1
                            c = h * 16 + (i - j)
                            act(pT, bank[:, u * 256:(u + 1) * 256], AF.Exp, bias=btab[:, c:c + 1])
                            if j == i:
                                p3 = pT.rearrange("p (m q) -> p m q", m=2)
                                vtt(p3, p3, bc_mid(tri_b, 2), ALU.mult)
                            for m in range(2):
                                mm(oacc[m][:, 0:129], pT[:, m * 128:(m + 1) * 128], Va[:, j, 0:129], j == 0, j == i)
                    e = i % 2
                    rd = st4[e]
                    vrecip(rd[:, 0:1], oacc[0][:, 128:129]); vrecip(rd[:, 1:2], oacc[1][:, 128:129])
                    vtt(rd[:, 1:2], rd[:, 1:2], lamt[:, 4:5], ALU.mult)
                    vts(odu[e], oacc[0][:, 0:128], rd[:, 0:1], None, ALU.mult, ALU.bypass)
                    vstt(od[e], oacc[1][:, 0:128], rd[:, 1:2], odu[e], ALU.mult, ALU.add)
                    vmemset(rd[:, 2:3], 0.0)
                    act(odu[e], od[e], AF.Square, accum=rd[:, 2:3])
                    act(rd[:, 3:4], rd[:, 2:3], AF.Ln, bias=EPS, scale=1.0 / 128)
                    act(rd[:, 3:4], rd[:, 3:4], AF.Exp, scale=-0.5)
                    vstt(odn[e], od[e], rd[:, 3:4], gsub_bc, ALU.mult, ALU.mult)
                    pt = bf(small())[:, 0:128]
                    tp(pt, odn[e], ident_b)
                    sg = stg[(i // 4) % 2]
                    vcopy(sg[:, (i % 4) * 128:(i % 4 + 1) * 128], pt)
                    if i % 4 == 3:
                        t0 = s * T + (i // 4) * 512
                        stt_(f"stg{(i // 4) % 2}", mix_d[h * 128:(h + 1) * 128, t0:t0 + 512], sg,
                             dwrites=[("mix", s, i // 4, h)])

            RG.reset()
            glow = RG.a(4096, BF16); gqT = RG.a(4096, BF16); gkT = RG.a(4096, BF16)
            gkt = RG.a(4096, BF16, "p (j n) -> p j n", j=16)
            gv = RG.a(8192, BF16, "p (j n) -> p j n", j=16)
            sgg = RG.a(16384, F32, "p (j n) -> p j n", j=16)
            l1 = [RG.a(512) for _ in range(2)]; ebs = [RG.a(512) for _ in range(2)]
            enb = [RG.a(512) for _ in range(2)]; ekd = [RG.a(512) for _ in range(2)]
            qb = [RG.a(256, BF16) for _ in range(2)]; kbb = [RG.a(256, BF16) for _ in range(2)]
            kd = [RG.a(256, BF16) for _ in range(2)]; atm = [RG.a(256, BF16) for _ in range(2)]
            S = RG.a(1024); Sb = [RG.a(512, BF16) for _ in range(2)]
            tg = [RG.a(1024) for _ in range(2)]; ogb = [RG.a(512, BF16) for _ in range(2)]
            junk2 = RM.a(1024)
            stg2 = [RM.a(2048, BF16, "p (r t) -> p r t", r=2) for _ in range(2)]
            st5 = [RM.a(64) for _ in range(2)]
            wl = wload([(6144, 16)])
            for tt in range(4):
                pb = big()
                for kc in range(KC):
                    mm(pb[0:16, :], wl[:, kc, 0:16], hT[:, kc, tt * 512:(tt + 1) * 512], kc == 0, kc == KC - 1)
                evac(glow[0:16, tt * 512:(tt + 1) * 512], pb[0:16, :])
            for hd in range(4):
                w1 = wload([(3072 + hd * 128, 128), (3584 + hd * 128, 128)])
                proj_fm(gqT, w1, 0, scale=float(128 ** -0.5))
                proj_fm(gkT, w1, 128)
                proj_tm(lambda kb, ps: evac(gkt[:, kb, :], ps), w1, 128, 128, small)
                w2 = wload([(4096 + hd * 256, 256)])
                proj_tm(lambda kb, ps: evac(gv[:, kb, :], ps), w2, 0, 256, mid)
                w3 = wload([(5120 + hd * 256, 256)])
                proj_tm(lambda kb, ps: act(sgg[:, kb, :], ps, AF.Silu), w3, 0, 256, mid)

                def prep(c):
                    e = c % 2
                    tok = slice(c * 128, (c + 1) * 128)
                    pl = small()
                    mm(pl, glow[0:16, tok], wgk[0:16, hd * 128:(hd + 1) * 128], True, False)
                    mm(pl, ones_b[0:1, 0:128], bgk[0:1, hd * 128:(hd + 1) * 128], False, True)
                    act(l1[e], pl, AF.Exp, scale=-1.0)
                    act(l1[e], l1[e], AF.Ln, bias=1.0)
                    pbt = small(); mm(pbt, l1[e], tri_f, True, True)
                    pu = small(); mm(pu, U_f, l1[e], True, True)
                    act(ebs[e], pbt, AF.Exp, scale=-1.0 / 16)
                    act(enb[e], pbt, AF.Exp, scale=1.0 / 16)
                    act(ekd[e], pu, AF.Exp, scale=-1.0 / 16)
                    vtt(qb[e], gqT[:, tok], ebs[e], ALU.mult)
                    vtt(kbb[e], gkT[:, tok], enb[e], ALU.mult)
                    vtt(kd[e], gkt[:, c, :], ekd[e], ALU.mult)

                def core(c):
                    e = c % 2
                    if c < 15:
                        pds = mid(); mm(pds, kd[e], gv[:, c, :], True, True)
                    pat = small(); mm(pat, kbb[e], qb[e], True, True)
                    vtt(atm[e], pat, tri_f, ALU.mult)
                    po = mid()
                    mm(po, atm[e], gv[:, c, :], True, c == 0)
                    if c > 0:
                        mm(po, qb[e], Sb[(c - 1) % 2], False, True)
                    if c < 15:
                        if c == 0:
                            vcopy(S, pds)
                        else:
                            vstt(S, S, ebs[e][:, 127:128], pds, ALU.mult, ALU.add)
                        act(Sb[c % 2], S, AF.Copy)
                    r5 = st5[e]
                    vmemset(r5[:, 0:1], 0.0)
                    act(junk2, po, AF.Square, accum=r5[:, 0:1])
                    act(r5[:, 1:2], r5[:, 0:1], AF.Ln, bias=EPS, scale=1.0 / 256)
                    act(r5[:, 1:2], r5[:, 1:2], AF.Exp, scale=-0.5)
                    vstt(tg[e], po, r5[:, 1:2], ggla_bc, ALU.mult, ALU.mult)
                    vtt(ogb[e], tg[e], sgg[:, c, :], ALU.mult)
                    sg = stg2[(c // 4) % 2]
                    for r in range(2):
                        pt = bf(small())[:, 0:128]
                        tp(pt, ogb[e][:, r * 128:(r + 1) * 128], ident_b)
                        evac(sg[:, r, (c % 4) * 128:(c % 4 + 1) * 128], pt)
                    if c % 4 == 3:
                        t0 = s * T + (c // 4) * 512
                        for r in range(2):
                            row = 1024 + hd * 256 + r * 128
                            stt_(f"stg2{(c // 4) % 2}{r}", mix_d[row:row + 128, t0:t0 + 512], sg[:, r, :],
                                 dwrites=[("mix", s, c // 4, row // 128)])

                prep(0)
                for c in range(16):
                    if c + 1 < 16:
                        prep(c + 1)
                    core(c)
                if MIXSTOP == f"gla{hd + 1}":
                    dbg_mix(1024, (hd + 1) * 256)
                    raise _Stop()

            RH.reset(); RW.reset(); RM.reset()
            mixa = RH.a(65536, BF16, "p (k t) -> p k t", k=16)
            wos = [RW.a(4096, BF16, "p (k n) -> p k n", k=16) for _ in range(4)]
            xrs = [RM.a(2048) for _ in range(3)]
            for tt in range(4):
                t0 = s * T + tt * 512
                ld(f"mixt{tt}", mixa[:, :, tt * 512:(tt + 1) * 512], mix_v[:, :, t0:t0 + 512],
                   dreads=[("mix", s, tt, r) for r in range(16)])
            n = 0
            for f in range(16):
                wo = wos[f % 4]
                ldc(f"wo{f % 4}", wo, w_o[l][:, f * 128:(f + 1) * 128].rearrange("(k p) n -> p k n", p=128))
                for tt in range(4):
                    t0 = s * T + tt * 512
                    xr = xrs[n % 3]
                    ld(f"xr{n % 3}", xr, xT_d[f * 128:(f + 1) * 128, t0:t0 + 512], dreads=[("xT", s, tt, f)])
                    pb = big()
                    for kc in range(KC):
                        mm(pb, wo[:, kc, :], mixa[:, kc, tt * 512:(tt + 1) * 512], kc == 0, kc == KC - 1)
                    vstt(xr, pb, modT[:, l, 32 + f, s:s + 1], xr, ALU.mult, ALU.add)
                    stt_(f"xs{n % 3}", xT_d[f * 128:(f + 1) * 128, t0:t0 + 512], xr, dwrites=[("xT", s, tt, f)])
                    n += 1

        def ffn(l, s):
            RH.reset(); RW.reset(); RD.reset(); RM.reset()
            hT = RH.a(65536, BF16, "p (k t) -> p k t", k=16)
            norm_phase(l, s, 1, hT)
            wus = [RW.a(8192, BF16, "p (k n) -> p k n", k=16) for _ in range(2)]
            gbs = [RD.a(514 * 4) for _ in range(2)]; t1s = [RD.a(2048) for _ in range(2)]
            t2s = [RD.a(2048) for _ in range(2)]; accs = [RD.a(4096, BF16) for _ in range(2)]
            n = 0
            for j in range(JC):
                wu = wus[j % 2]
                ldc(f"wu{j % 2}0", wu[:, :, 0:128], w_up[l][:, j * 128:(j + 1) * 128].rearrange("(k p) n -> p k n", p=128))
                ldc(f"wu{j % 2}1", wu[:, :, 128:256], w_up[l][:, DFF + j * 128:DFF + (j + 1) * 128].rearrange("(k p) n -> p k n", p=128))
                acc = accs[j % 2]
                for tt in range(4):
                    pu = big()
                    for kc in range(KC):
                        mm(pu, wu[:, kc, 0:128], hT[:, kc, tt * 512:(tt + 1) * 512], kc == 0, kc == KC - 1)
                    pg = big()
                    for kc in range(KC):
                        mm(pg, wu[:, kc, 128:256], hT[:, kc, tt * 512:(tt + 1) * 512], kc == 0, kc == KC - 1)
                    gb = gbs[n % 2]; t1 = t1s[n % 2]; t2 = t2s[n % 2]
                    if tt == 0:
                        vmemset(gb[:, 0:2], 0.0)
                    else:
                        vcopy(gb[:, 0:2], gbs[(n - 1) % 2][:, 512:514])
                    act(gb[:, 2:514], pg, AF.Copy)
                    vts(t1, gb[:, 2:514], wcv[:, 2, j:j + 1], bcv[:, j:j + 1], ALU.mult, ALU.add)
                    vstt(t1, gb[:, 1:513], wcv[:, 1, j:j + 1], t1, ALU.mult, ALU.add)
                    vstt(t1, gb[:, 0:512], wcv[:, 0, j:j + 1], t1, ALU.mult, ALU.add)
                    act(t2, t1, AF.Gelu)
                    vtt(acc[:, tt * 512:(tt + 1) * 512], t2, pu, ALU.mult)
                    n += 1
                stt_(f"acc{j % 2}", act_d[j * 128:(j + 1) * 128, s * T:(s + 1) * T], acc, dwrites=[("act", s, j)])
            RHW.reset(); RD.reset(); RM.reset()
            at = RHW.a(JC * 1024 * 2, BF16, "p (j t) -> p j t", j=JC)
            wds = [RD.a(JC * 128 * 2, BF16, "p (j n) -> p j n", j=JC) for _ in range(3)]
            xrs = [RM.a(2048) for _ in range(3)]
            n = 0
            for half in range(2):
                for q in range(2):
                    t0 = s * T + half * 1024 + q * 512
                    ld(f"at{q}", at[:, :, q * 512:(q + 1) * 512], act_v[:, :, t0:t0 + 512],
                       dreads=[("act", s, j) for j in range(JC)])
                for f in range(16):
                    wd = wds[f % 3]
                    ldc(f"wd{f % 3}", wd, w_dn[l][:, f * 128:(f + 1) * 128].rearrange("(j p) n -> p j n", p=128))
                    for q in range(2):
                        tt = half * 2 + q
                        t0 = s * T + tt * 512
                        xr = xrs[n % 3]
                        ld(f"xr{n % 3}", xr, xT_d[f * 128:(f + 1) * 128, t0:t0 + 512], dreads=[("xT", s, tt, f)])
                        pb = big()
                        for j in range(JC):
                            mm(pb, wd[:, j, :], at[:, j, q * 512:(q + 1) * 512], j == 0, j == JC - 1)
                        vstt(xr, pb, modT[:, l, 80 + f, s:s + 1], xr, ALU.mult, ALU.add)
                        stt_(f"xs{n % 3}", xT_d[f * 128:(f + 1) * 128, t0:t0 + 512], xr, dwrites=[("xT", s, tt, f)])
                        n += 1

        def final():
            RG.reset(); RD.reset()
            xins = [RG.a(16384, F32, "p (k t) -> p k t", k=16) for _ in range(2)]
            sq = RG.a(8192, BF16, "p (k t) -> p k t", k=16)
            rss = [RG.a(1024) for _ in range(2)]
            ystg = [RD.a(8192) for _ in range(2)]
            n = 0
            for s in range(NSQ):
                for tt in range(8):
                    xin = xins[tt % 2]; rs = rss[tt % 2]
                    t0 = s * T + tt * 256
                    ld(f"xin{tt % 2}", xin, xT_v[:, :, t0:t0 + 256], dreads=[("xT", s, tt // 2, f) for f in range(16)])
                    rms_stats(xin, sq, rs, 256, 1.0 / D)
                    vtt(xin, xin, bc_mid(rs, 16), ALU.mult)
                    for k in range(KC):
                        act(xin[:, k, :], xin[:, k, :], AF.Copy, scale=gfin[:, k:k + 1])
                    for sub in range(2):
                        ys = ystg[n % 2]
                        for g in range(4):
                            pb = big()
                            for q in range(4):
                                k = g * 4 + q
                                tp(pb[:, q * 128:(q + 1) * 128], xin[:, k, sub * 128:(sub + 1) * 128], ident_f)
                            evac(ys[:, g * 512:(g + 1) * 512], pb)
                        r0 = t0 + sub * 128
                        stt_(f"ystg{n % 2}", y_d[r0:r0 + 128, :], ys)
                        n += 1

        stages = []
        for l in range(2):
            stages.append(("setup", l, None))
            for s in range(NSQ):
                stages.append(("mix", l, s))
            for s in range(NSQ):
                stages.append(("ffn", l, s))
        done = False
        if stop_after == ("conv", 0, None):
            for blk in range(NTOK // 128):
                conv_block(blk)
            stages = []; done = True
        else:
            mod_phase(True)
        if stop_after == ("mod", 0, None):
            stages = []; done = True
        try:
            for (kind, l, s) in stages:
                if kind == "setup":
                    layer_setup(l)
                elif kind == "mix":
                    mixing(l, s)
                else:
                    ffn(l, s)
                if stop_after == (kind, l, s):
                    done = True
                    break
            if not done:
                final()
            if dbg_d is not None:
                dump_and_finish()
        except _Stop:
            pass
        P.wait_all("sp")
        counts = P.emit(); counts["ntrk"] = len(P.cnt)
    return nc, counts


def _consts():
    p = np.arange(128)
    ident = np.eye(128, dtype=np.float32)
    tri = (p[:, None] <= p[None, :]).astype(np.float32)
    U = (p[:, None] > p[None, :]).astype(np.float32)
    slopes = np.array([(2.0 ** (-8.0 / 8)) ** (i + 1) for i in range(8)], dtype=np.float64)
    bt = np.zeros((128, 128), np.float32)
    for h in range(8):
        for d in range(16):
            bt[:, h * 16 + d] = slopes[h] * (p - 64 - 128 * d)
    return np.concatenate([ident, tri, U, bt], axis=1).astype(np.float32)


_CACHE = {}


def kernel(**inputs):
    stop_after = inputs.pop("_stop_after", None)
    key = ("nc", stop_after)
    if key not in _CACHE:
        _CACHE[key] = build_program(stop_after)
    nc, counts = _CACHE[key]
    f = lambda a: np.ascontiguousarray(np.asarray(a, dtype=np.float32))
    shared = {k: f(inputs[k]) for k in ("w_ada", "b_ada", "g_mix_norm", "w_in", "g_diff_subln", "w_gk_up", "b_gk",
                                        "g_gla_norm", "w_o", "g_ffn_norm", "w_up", "w_conv", "b_conv", "w_down",
                                        "g_final")}
    shared["diff_lambda"] = f(inputs["diff_lambda"]).reshape(2, 256)
    shared["consts"] = _consts()
    x = f(inputs["x"]); c = f(inputs["c"])
    ncores = int(inputs.pop("_ncores", 8)) if "_ncores" in inputs else 8
    in_maps = []
    for i in range(ncores):
        m = dict(shared)
        m["x"] = x[2 * i:2 * i + 2].reshape(NTOK, D)
        m["c"] = c[2 * i:2 * i + 2]
        in_maps.append(m)
    res = run_bass_kernel_spmd(nc, in_maps, core_ids=list(range(ncores)))
    if stop_after is not None:
        return [r["dbg"] for r in res.results]
    out = np.stack([r["y"].reshape(NSQ, T, D) for r in res.results], axis=0).reshape(ncores * NSQ, T, D)
    return out.astype(np.float32)
```
